# Optimizing a Trainium2 kernel written in Bass

```python
import jax, jax.numpy as jnp
from jax import lax
import numpy as np

D_MODEL = 1024
BATCH = 4
SEQ = 4096
DEPTH = 2

HEAD_DIM = 64
ROT_DIM = HEAD_DIM // 4
ROPE_THETA = 500000.0
A_HEADS = (D_MODEL // 2) // (2 * HEAD_DIM)
B_HEADS = (D_MODEL // 2) // HEAD_DIM
KV_LORA = D_MODEL // 4
IDX_HEADS = 4
IDX_DIM = 64
TOPK_MAX = 256
QBLOCK = 128
C_WIDTH = D_MODEL // 2
CONV_W = 3
POOL_WINDOWS = (2, 4, 8, 16)
POOL_WIDTH = D_MODEL // 2
POOL_GROUP = POOL_WIDTH // len(POOL_WINDOWS)
MEM_LEN = 256
X_HEADS = 4
X_HEAD_DIM = D_MODEL // 8
D_FF = 4 * D_MODEL
EPS = 1e-6
N_EVEN = (DEPTH + 1) // 2
N_ODD = DEPTH // 2
EVEN_SPLITS = (A_HEADS * 2 * HEAD_DIM, A_HEADS * 2 * HEAD_DIM, A_HEADS * 2 * HEAD_DIM,
               B_HEADS * HEAD_DIM, KV_LORA, IDX_HEADS * IDX_DIM, IDX_DIM, IDX_HEADS)
EVEN_IN = sum(EVEN_SPLITS)
ODD_SPLITS = (C_WIDTH, C_WIDTH, C_WIDTH, POOL_WIDTH)
ODD_IN = sum(ODD_SPLITS)

kernel_name = 'hybrid_diff_dsa_conv_pool_trunk'


def rmsnorm(x, g):
    xf = x.astype(jnp.float32)
    y = xf * lax.rsqrt(jnp.mean(xf * xf, axis=-1, keepdims=True) + EPS)
    return (y * g.astype(jnp.float32)).astype(x.dtype)


def _split(a, sizes):
    return jnp.split(a, np.cumsum(sizes)[:-1].tolist(), axis=-1)


def rope_tables(positions):
    inv = ROPE_THETA ** (-jnp.arange(0, ROT_DIM, 2, dtype=jnp.float32) / ROT_DIM)
    ang = positions.astype(jnp.float32)[..., None] * inv
    return jnp.cos(ang)[:, :, None, :], jnp.sin(ang)[:, :, None, :]


def rope(x, cos, sin):
    half = ROT_DIM // 2
    xr, xp = x[..., :ROT_DIM], x[..., ROT_DIM:]
    x1, x2 = xr[..., :half], xr[..., half:]
    rot = jnp.concatenate([x1 * cos - x2 * sin, x2 * cos + x1 * sin], axis=-1)
    return jnp.concatenate([rot.astype(x.dtype), xp], axis=-1)


def even_mixer(h, cos, sin, w_in, gq_a, gk_a, lam, lam_init, g_sub_a,
               g_kv_b, w_kv_up_b, gq_b, gk_b, g_kidx, w_out, k_sel):
    B, S, _ = h.shape
    nb = S // QBLOCK

    def to_blocks(a):
        return jnp.moveaxis(a.reshape((B, nb, QBLOCK) + a.shape[2:]), 1, 0)

    def from_blocks(o):
        return jnp.moveaxis(o, 0, 1).reshape((B, S) + o.shape[3:])

    qa, ka, va, qb, ckv, qi, ki, wi = _split(h @ w_in, EVEN_SPLITS)

    qa = rope(rmsnorm(qa.reshape(B, S, 2 * A_HEADS, HEAD_DIM), gq_a), cos, sin)
    qa = qa.reshape(B, S, A_HEADS, 2, HEAD_DIM)
    ka = rope(rmsnorm(ka.reshape(B, S, 2 * A_HEADS, HEAD_DIM), gk_a), cos, sin)
    ka = ka.reshape(B, S, A_HEADS, 2, HEAD_DIM)
    va = va.reshape(B, S, A_HEADS, 2 * HEAD_DIM)

    kb, vb = jnp.split(rmsnorm(ckv, g_kv_b) @ w_kv_up_b, 2, axis=-1)
    qb = rope(rmsnorm(qb.reshape(B, S, B_HEADS, HEAD_DIM), gq_b), cos, sin)
    kb = rope(rmsnorm(kb.reshape(B, S, B_HEADS, HEAD_DIM), gk_b), cos, sin)
    vb = vb.reshape(B, S, B_HEADS, HEAD_DIM)
    qi = rope(qi.reshape(B, S, IDX_HEADS, IDX_DIM), cos, sin)
    ki = rope(rmsnorm(ki, g_kidx)[:, :, None, :], cos, sin)[:, :, 0, :]
    wi = wi.astype(jnp.float32) * (IDX_HEADS ** -0.5 * IDX_DIM ** -0.5)

    kpos = jnp.arange(S)
    scale = HEAD_DIM ** -0.5

    def a_block(args):
        q, t0 = args
        qpos = t0 + jnp.arange(QBLOCK)
        causal = kpos[None, :] <= qpos[:, None]
        logits = jnp.einsum('bqhcd,bkhcd->bhcqk', q, ka).astype(jnp.float32) * scale
        p = jax.nn.softmax(jnp.where(causal, logits, -jnp.inf), axis=-1)
        attn = p[:, :, 0] - lam * p[:, :, 1]
        return jnp.einsum('bhqk,bkhe->bqhe', attn.astype(va.dtype), va)

    def b_block(args):
        q, qidx, widx, t0 = args
        qpos = t0 + jnp.arange(QBLOCK)
        causal = kpos[None, :] <= qpos[:, None]
        isc = jax.nn.relu(jnp.einsum('bqhd,bkd->bqhk', qidx, ki).astype(jnp.float32))
        isc = jnp.einsum('bqhk,bqh->bqk', isc, widx)
        isc = jnp.where(causal[None], isc, -jnp.inf)
        _, idx = lax.top_k(isc, k_sel)
        k_g = jax.vmap(lambda a, i: a[i])(kb, idx)
        v_g = jax.vmap(lambda a, i: a[i])(vb, idx)
        logits = jnp.einsum('bqhd,bqkhd->bhqk', q, k_g).astype(jnp.float32) * scale
        valid = (idx <= qpos[None, :, None])[:, None]
        p = jax.nn.softmax(jnp.where(valid, logits, -jnp.inf), axis=-1)
        return jnp.einsum('bhqk,bqkhd->bqhd', p.astype(vb.dtype), v_g)

    starts = jnp.arange(nb, dtype=jnp.int32) * QBLOCK
    out_a = from_blocks(lax.map(a_block, (to_blocks(qa), starts)))
    out_a = rmsnorm(out_a, g_sub_a) * (1.0 - lam_init)
    out_b = from_blocks(lax.map(b_block, (to_blocks(qb), to_blocks(qi), to_blocks(wi), starts)))
    y = jnp.concatenate([out_a.reshape(B, S, -1), out_b.reshape(B, S, -1)], axis=-1)
    return y @ w_out


def pool_mixer(z, w_pool, pool_scale):
    B, S, _ = z.shape
    zf = z.astype(jnp.float32).reshape(B, S, len(POOL_WINDOWS), POOL_GROUP)
    csp = jnp.pad(jnp.cumsum(zf, axis=1), ((0, 0), (1, 0), (0, 0), (0, 0)))
    t = jnp.arange(S)
    outs = []
    for g, w in enumerate(POOL_WINDOWS):
        lo = jnp.maximum(t + 1 - w, 0)
        win_sum = csp[:, 1:, g] - csp[:, lo, g]
        cnt = jnp.minimum(t + 1, w).astype(jnp.float32)
        outs.append(win_sum / cnt[None, :, None] - zf[:, :, g])
    pooled = jnp.stack(outs, axis=2).astype(z.dtype)
    y = jnp.einsum('bsgc,gcd->bsgd', pooled, w_pool).reshape(B, S, POOL_WIDTH)
    return y * pool_scale


def odd_mixer(h, w_in, conv_w, w_pool, pool_scale, w_out):
    gb, gc, hc, zd = _split(h @ w_in, ODD_SPLITS)
    u = gc * hc
    conv = lax.conv_general_dilated(
        u, conv_w[:, None, :].astype(u.dtype), window_strides=(1,),
        padding=[(CONV_W - 1, 0)], dimension_numbers=('NWC', 'WIO', 'NWC'),
        feature_group_count=C_WIDTH)
    yc = gb * conv
    yd = pool_mixer(zd, w_pool, pool_scale)
    return jnp.concatenate([yc, yd], axis=-1) @ w_out


def cross_attn(h, m, wq, wkv, gq, gk, wo):
    B, S, _ = h.shape
    q = rmsnorm((h @ wq).reshape(B, S, X_HEADS, X_HEAD_DIM), gq)
    k, v = jnp.split(m @ wkv, 2, axis=-1)
    k = rmsnorm(k.reshape(B, -1, X_HEADS, X_HEAD_DIM), gk)
    v = v.reshape(B, -1, X_HEADS, X_HEAD_DIM)
    logits = jnp.einsum('bqhd,bkhd->bhqk', q, k).astype(jnp.float32) * X_HEAD_DIM ** -0.5
    p = jax.nn.softmax(logits, axis=-1)
    o = jnp.einsum('bhqk,bkhd->bqhd', p.astype(v.dtype), v).reshape(B, S, -1)
    return o @ wo


def mlp(h, w_up, w_down):
    return jnp.square(jax.nn.relu(h @ w_up)) @ w_down


def setup_inputs(seed: int = 0) -> dict:
    key = jax.random.key(seed)
    ks = iter(jax.random.split(key, 48))

    def nrm(shape, scale):
        return scale * jax.random.normal(next(ks), shape, jnp.float32)

    def gain(shape):
        return 1.0 + 0.05 * jax.random.normal(next(ks), shape, jnp.float32)

    d = D_MODEL
    return {
        'x': nrm((BATCH, SEQ, d), 1.0),
        'mem': nrm((BATCH, MEM_LEN, d), 1.0),
        'positions': jnp.tile(jnp.arange(SEQ, dtype=jnp.int32)[None, :], (BATCH, 1)),
        'g_mix': gain((DEPTH, d)),
        'g_xattn': gain((DEPTH, d)),
        'g_mem': gain((DEPTH, d)),
        'g_mlp': gain((DEPTH, d)),
        'wq_x': nrm((DEPTH, d, X_HEADS * X_HEAD_DIM), d ** -0.5),
        'wkv_x': nrm((DEPTH, d, 2 * X_HEADS * X_HEAD_DIM), d ** -0.5),
        'gq_x': gain((DEPTH, X_HEAD_DIM)),
        'gk_x': gain((DEPTH, X_HEAD_DIM)),
        'wo_x': nrm((DEPTH, X_HEADS * X_HEAD_DIM, d), (X_HEADS * X_HEAD_DIM) ** -0.5),
        'w_up': nrm((DEPTH, d, D_FF), d ** -0.5),
        'w_down': nrm((DEPTH, D_FF, d), D_FF ** -0.5),
        'w_in_e': nrm((N_EVEN, d, EVEN_IN), d ** -0.5),
        'gq_a': gain((N_EVEN, HEAD_DIM)),
        'gk_a': gain((N_EVEN, HEAD_DIM)),
        'lam_q1': nrm((N_EVEN, HEAD_DIM), 0.1),
        'lam_k1': nrm((N_EVEN, HEAD_DIM), 0.1),
        'lam_q2': nrm((N_EVEN, HEAD_DIM), 0.1),
        'lam_k2': nrm((N_EVEN, HEAD_DIM), 0.1),
        'g_sub_a': gain((N_EVEN, 2 * HEAD_DIM)),
        'g_kv_b': gain((N_EVEN, KV_LORA)),
        'w_kv_up_b': nrm((N_EVEN, KV_LORA, 2 * B_HEADS * HEAD_DIM), KV_LORA ** -0.5),
        'gq_b': gain((N_EVEN, HEAD_DIM)),
        'gk_b': gain((N_EVEN, HEAD_DIM)),
        'g_kidx': gain((N_EVEN, IDX_DIM)),
        'w_out_e': nrm((N_EVEN, d, d), d ** -0.5),
        'w_in_o': nrm((N_ODD, d, ODD_IN), d ** -0.5),
        'conv_w': nrm((N_ODD, CONV_W, C_WIDTH), CONV_W ** -0.5),
        'w_pool': nrm((N_ODD, len(POOL_WINDOWS), POOL_GROUP, POOL_GROUP), POOL_GROUP ** -0.5),
        'pool_scale': gain((N_ODD, POOL_WIDTH)),
        'w_out_o': nrm((N_ODD, d, d), d ** -0.5),
    }


def reference(x, mem, positions, g_mix, g_xattn, g_mem, g_mlp, wq_x, wkv_x, gq_x, gk_x,
              wo_x, w_up, w_down, w_in_e, gq_a, gk_a, lam_q1, lam_k1, lam_q2, lam_k2,
              g_sub_a, g_kv_b, w_kv_up_b, gq_b, gk_b, g_kidx, w_out_e, w_in_o, conv_w,
              w_pool, pool_scale, w_out_o):
    S = x.shape[1]
    k_sel = min(TOPK_MAX, S // 4)
    cos, sin = rope_tables(positions)
    for i in range(DEPTH):
        j = i // 2
        h = rmsnorm(x, g_mix[i])
        if i % 2 == 0:
            lam_init = 0.8 - 0.6 * float(np.exp(-0.3 * i))
            f32 = jnp.float32
            lam = (jnp.exp(jnp.sum(lam_q1[j].astype(f32) * lam_k1[j].astype(f32)))
                   - jnp.exp(jnp.sum(lam_q2[j].astype(f32) * lam_k2[j].astype(f32)))
                   + lam_init)
            mix = even_mixer(h, cos, sin, w_in_e[j], gq_a[j], gk_a[j], lam, lam_init,
                             g_sub_a[j], g_kv_b[j], w_kv_up_b[j], gq_b[j], gk_b[j],
                             g_kidx[j], w_out_e[j], k_sel)
        else:
            mix = odd_mixer(h, w_in_o[j], conv_w[j], w_pool[j], pool_scale[j], w_out_o[j])
        x = x + mix
        x = x + cross_attn(rmsnorm(x, g_xattn[i]), rmsnorm(mem, g_mem[i]),
                           wq_x[i], wkv_x[i], gq_x[i], gk_x[i], wo_x[i])
        x = x + mlp(rmsnorm(x, g_mlp[i]), w_up[i], w_down[i])
    return x
```

```python
import numpy as np
from contextlib import ExitStack
import concourse.bass as bass
import concourse.mybir as mybir
from concourse.bass_utils import run_bass_kernel_spmd

F32 = mybir.dt.float32
BF16 = mybir.dt.bfloat16
I32 = mybir.dt.int32
AF = mybir.ActivationFunctionType
ALU = mybir.AluOpType
AX = mybir.AxisListType

D = 1024
NT = 16
NB = 32
EPS = 1e-6
NEG = -1.0e30
BIS_ITERS = 15
MASK_ENG = "pool"
TOPK = 256


class Sem:
    def __init__(self, h, name):
        self.h = h
        self.cnt = 0
        self.name = name


class Tok:
    __slots__ = ("w", "r")

    def __init__(self):
        self.w = {}
        self.r = {}


def _mx(d, s):
    for k, v in s.items():
        if d.get(k, 0) < v:
            d[k] = v


class KB:
    def __init__(self, nc):
        self.nc = nc
        self.es = ExitStack()
        self.engs = {"pe": nc.tensor, "act": nc.scalar, "dve": nc.vector,
                     "pool": nc.gpsimd, "sp": nc.sync}
        self.allsems = []
        self.esem = {k: self.newsem("s_" + k) for k in self.engs}
        self.waited = {k: {} for k in self.engs}
        self.nins = 0

    def newsem(self, name):
        h = self.es.enter_context(self.nc.semaphore(name))
        s = Sem(h, name)
        self.allsems.append(s)
        return s

    def _wait(self, e, deps):
        w = self.waited[e]
        for s, v in deps.items():
            if e == "pe" and s is self.esem["pe"]:
                continue
            if w.get(s, 0) >= v:
                continue
            self.engs[e].wait_ge(s.h, v)
            w[s] = v
            self.nins += 1

    def _deps(self, reads, writes):
        deps = {}
        for t in reads:
            _mx(deps, t.w)
        for t in writes:
            _mx(deps, t.w)
            _mx(deps, t.r)
        return deps

    def _mark(self, s, reads, writes, merge):
        for t in reads:
            t.r[s] = s.cnt
        for t in writes:
            if merge:
                t.w[s] = s.cnt
            else:
                t.w = {s: s.cnt}
                t.r = {}

    def op(self, e, fn, reads=(), writes=(), merge=False):
        self._wait(e, self._deps(reads, writes))
        ins = fn(self.engs[e])
        s = self.esem[e]
        s.cnt += 1
        ins.then_inc(s.h, 1)
        self.nins += 1
        self._mark(s, reads, writes, merge)

    def dma(self, q, out, in_, dsem, reads=(), writes=(), merge=False):
        self._wait(q, self._deps(reads, writes))
        ins = self.engs[q].dma_start(out=out, in_=in_)
        dsem.cnt += 16
        ins.then_inc(dsem.h, 16)
        self.nins += 1
        self._mark(dsem, reads, writes, merge)

    def collective(self, kind, groups, in_ap, out_ap, csem, reads=(), writes=()):
        self._wait("pool", self._deps(reads, writes))
        ins = self.nc.gpsimd.collective_compute(kind, ALU.bypass, replica_groups=groups, ins=[in_ap], outs=[out_ap])
        csem.cnt += 1
        ins.then_inc(csem.h)
        self.nins += 1
        self._mark(csem, reads, writes, False)

    def flush(self, dsem, toks):
        for t in toks:
            for d in list(t.w):
                if d is dsem or d.name.startswith("d_misc"):
                    t.w[d] = d.cnt

    def barrier(self):
        for e in self.engs:
            deps = {s: s.cnt for s in self.allsems if s.cnt > 0}
            self._wait(e, deps)


class Ctx:
    pass


def build(mode="ALL", nt=NT, topk=TOPK, groups=None):
    nc = bass.Bass("TRN2", target_bir_lowering=False)
    kb = KB(nc)
    c = Ctx()
    c.nc, c.kb, c.nt, c.topk = nc, kb, nt, topk
    c.groups = groups if groups is not None else [[0, 1], [2, 3], [4, 5], [6, 7]]
    ntok = nt * 128
    nl = 2 if mode == "ALL" else 1
    c.li = (lambda l: l) if mode == "ALL" else (lambda l: 0)
    has0 = mode in ("L0mix", "L0", "ALL")
    has1 = mode in ("L1", "ALL")

    def din(name, shape, dt=F32):
        return nc.dram_tensor(name, list(shape), dt, kind="ExternalInput").ap()

    c.xo = din("xo", [ntok, D])
    c.ident = din("ident", [128, 128])
    c.g_mix = din("g_mix", [nl, D])
    if mode != "L0mix":
        c.mem = din("mem", [256, D])
        c.g_xattn = din("g_xattn", [nl, D])
        c.g_mem = din("g_mem", [nl, D])
        c.g_mlp = din("g_mlp", [nl, D])
        c.wq_x = din("wq_x", [nl, D, 512])
        c.wkv_x = din("wkv_x", [nl, D, 1024])
        c.gq_x = din("gq_x", [nl, 128])
        c.gk_x = din("gk_x", [nl, 128])
        c.wo_x = din("wo_x", [nl, 512, D])
        c.w_up = din("w_up", [nl, D, 4096])
        c.w_down = din("w_down", [nl, 4096, D])
    if has0:
        c.xt = din("xt", [ntok, D])
        c.pos = din("pos", [128, 2 * nt], I32)
        c.invf = din("invf", [128, 16])
        c.tri = din("tri", [128, 128])
        c.omask = din("omask", [128, 128])
        c.trineg = din("trineg", [128, 128])
        c.oneg = din("oneg", [128, 128])
        c.lamv = din("lamv", [4, 64])
        c.gq_a = din("gq_a", [1, 64]); c.gk_a = din("gk_a", [1, 64]); c.g_sub_a = din("g_sub_a", [1, 128])
        c.g_kv_b = din("g_kv_b", [1, 256]); c.gq_b = din("gq_b", [1, 64]); c.gk_b = din("gk_b", [1, 64])
        c.g_kidx = din("g_kidx", [1, 64])
        c.wA_k = din("wA_k", [D, 1024]); c.wA_q = din("wA_q", [D, 512])
        c.wB_k = din("wB_k", [D, 320]); c.wB_q = din("wB_q", [D, 772])
        c.w_kv_up = din("w_kv_up", [256, 1024]); c.w_out_e = din("w_out_e", [D, D])
    if has1:
        c.w_in_o = din("w_in_o", [D, 2048])
        c.conv_w = din("conv_w", [128, 12])
        c.w_pool = din("w_pool", [4, 128, 128])
        c.pool_scale = din("pool_scale", [128, 4])
        c.w_out_o = din("w_out_o", [D, D])
        c.invc = din("invc", [128, 64])
    if mode == "L1":
        c.xh = din("xh", [256, D])
    if mode == "ALL":
        c.hsel = din("hsel", [128, 2])
        c.tails = nc.dram_tensor("tails", [nt * 16, D], F32)
        c.gath = nc.dram_tensor("gath", [2 * nt * 16, D], F32)
    c.out = nc.dram_tensor("out", [ntok, D], F32, kind="ExternalOutput").ap()
    out_v = c.out.rearrange("(t p) d -> p t d", p=128)
    if mode == "L0mix":
        c.x1s_v = out_v
    elif has0:
        c.x1s = nc.dram_tensor("x1s", [ntok, D], F32).ap()
        c.x1s_v = c.x1s.rearrange("(t p) d -> p t d", p=128)
    c.x1s_t = [Tok() for _ in range(nt)]

    es = kb.es
    with es:
        c.uid = 0

        def sb(name, shape, dt, stack=es):
            c.uid += 1
            return stack.enter_context(nc.sbuf_tensor(f"{name}_{c.uid}", list(shape), dt))

        c.sb = sb
        c.T = []
        c.Tt = []
        for i in range(2):
            c.T.append(es.enter_context(nc.psum_tensor(f"psT{i}", [128, 1024], BF16)))
            c.Tt.append(Tok())
        c.M = []
        c.Mt = []
        for i in range(6):
            c.M.append(es.enter_context(nc.psum_tensor(f"psM{i}", [128, 512], F32)))
            c.Mt.append(Tok())
        c.ti = 0
        c.mi = 0
        c.identb = sb("identb", [128, 128], BF16)
        c.ident_t = Tok()
        c.dsem_misc = kb.newsem("d_misc")
        c.dsem_misc_sw = kb.newsem("d_misc_sw")
        c.misc = [c.ident_t]
        c.dsem_in = [kb.newsem("d_in0"), kb.newsem("d_in1")]
        c.dsem_x1 = [kb.newsem("d_x1a"), kb.newsem("d_x1b")]
        kb.dma("pool", c.identb[:], c.ident[:, :], c.dsem_misc_sw, writes=[c.ident_t])
        c.neghalf = sb("neghalf", [128, 16], F32)
        c.neghalf_t = Tok()
        kb.op("pool", lambda e: e.memset(c.neghalf[:], -0.5), writes=[c.neghalf_t])
        c.dsem_x = kb.newsem("d_x")
        c.dsem_w = [kb.newsem(f"d_w{i}") for i in range(8)]
        c.dsem_out = kb.newsem("d_out")

        if has0:
            l0_mixer(c, mode)
        if mode != "L0mix":
            c.X = sb("X", [128, nt, D], F32)
            c.Xt = [Tok() for _ in range(nt)]
            if mode == "L1":
                xo_v = c.xo.rearrange("(t p) d -> p t d", p=128)
                for t in range(nt):
                    kb.dma("sp", c.X[:, t, :], xo_v[:, t, :], c.dsem_x, writes=[c.Xt[t]])
            else:
                for t in range(nt):
                    kb.dma("sp", c.X[:, t, :], c.x1s_v[:, t, :], c.dsem_x, reads=[c.x1s_t[t]], writes=[c.Xt[t]])
            kb.flush(c.dsem_x, c.Xt)
            if has0:
                xattn(c, 0)
                mlp(c, 0)
            if has1:
                l1_mixer(c, mode)
                xattn(c, 1)
                mlp(c, 1)
            for t in range(nt):
                kb.dma("sp", out_v[:, t, :], c.X[:, t, :], c.dsem_out, reads=[c.Xt[t]])
        kb.barrier()
    c.nins = kb.nins
    return nc, c


def pipeline(n, stages):
    k = len(stages)
    for step in range(n + k - 1):
        for si in range(k):
            t = step - si
            if 0 <= t < n:
                stages[si](t)


def next_T(c):
    i = c.ti
    c.ti = (c.ti + 1) % 2
    return c.T[i], c.Tt[i]


def next_M(c, lo=0, hi=6):
    if c.mi < lo or c.mi >= hi:
        c.mi = lo
    i = c.mi
    c.mi = c.mi + 1
    if c.mi >= hi:
        c.mi = lo
    return c.M[i], c.Mt[i]


def load_bc(c, stack, name, src_row, n, q="sp"):
    t = c.sb(name, [128, n], F32, stack)
    tok = Tok()
    c.kb.dma(q, t[:], src_row.partition_broadcast(128), c.dsem_misc, writes=[tok])
    c.misc.append(tok)
    return t, tok


def rmsnorm_tile(c, x_ap, x_tok, g_ap, g_tok, out_ap, out_tok, n, scr, scr_tok, st, st_tok):
    kb = c.kb
    kb.op("act", lambda e: e.activation(out=scr[:, :n], in_=x_ap, func=AF.Square, accum_out=st[:, 0:1]),
          reads=[x_tok], writes=[scr_tok, st_tok])
    kb.op("dve", lambda e: e.tensor_scalar(out=st[:, 1:2], in0=st[:, 0:1], scalar1=1.0 / n, scalar2=EPS,
                                           op0=ALU.mult, op1=ALU.add),
          reads=[st_tok], writes=[st_tok])
    npar = x_ap.shape[0]
    kb.op("pool", lambda e: e.tensor_tensor(out=st[:, 3:4], in0=st[:, 1:2], in1=c.neghalf[:npar, 0:1], op=ALU.pow),
          reads=[st_tok, c.neghalf_t], writes=[st_tok])
    kb.op("dve", lambda e: e.scalar_tensor_tensor(out=out_ap, in0=x_ap, scalar=st[:, 3:4], in1=g_ap,
                                                  op0=ALU.mult, op1=ALU.mult),
          reads=[x_tok, st_tok, g_tok], writes=[out_tok])


def headnorm(c, src_ap, src_tok, H, Dh, g_ap, g_tok, out_ap, out_tok, scr, scr_tok, st, st_tok, extra_scale=1.0):
    kb = c.kb
    n = H * Dh
    scr3 = scr[:, :n].rearrange("p (h d) -> p h d", h=H)
    kb.op("dve", lambda e: e.tensor_tensor(out=scr3, in0=src_ap, in1=src_ap, op=ALU.mult),
          reads=[src_tok], writes=[scr_tok])
    kb.op("dve", lambda e: e.reduce_sum(out=st[:, 0:H], in_=scr3, axis=AX.X),
          reads=[scr_tok], writes=[st_tok])
    kb.op("dve", lambda e: e.tensor_scalar(out=st[:, H:2 * H], in0=st[:, 0:H], scalar1=1.0 / Dh, scalar2=EPS,
                                           op0=ALU.mult, op1=ALU.add),
          reads=[st_tok], writes=[st_tok])
    kb.op("pool", lambda e: e.tensor_tensor(out=st[:, 3 * H:4 * H], in0=st[:, H:2 * H], in1=c.neghalf[:, 0:H], op=ALU.pow),
          reads=[st_tok, c.neghalf_t], writes=[st_tok])
    rb = st[:, 3 * H:4 * H].unsqueeze(2).broadcast_to([128, H, Dh])
    kb.op("dve", lambda e: e.tensor_tensor(out=scr3, in0=src_ap, in1=rb, op=ALU.mult),
          reads=[src_tok, st_tok], writes=[scr_tok])
    gb = g_ap.unsqueeze(1).broadcast_to([128, H, Dh])
    if extra_scale == 1.0:
        kb.op("dve", lambda e: e.tensor_tensor(out=out_ap, in0=scr3, in1=gb, op=ALU.mult),
              reads=[scr_tok, g_tok], writes=[out_tok])
    else:
        kb.op("dve", lambda e: e.scalar_tensor_tensor(out=out_ap, in0=scr3, scalar=float(extra_scale), in1=gb,
                                                      op0=ALU.mult, op1=ALU.mult),
              reads=[scr_tok, g_tok], writes=[out_tok])


def transposes(c, srcs, src_tok, dst_ap, dst_tok, eng="act", merge=False, halves=False):
    kb = c.kb
    T, Tt = next_T(c)
    n = len(srcs)
    P = srcs[0].shape[0]
    Q = srcs[0].shape[1]
    for i, s in enumerate(srcs):
        kb.op("pe", lambda e, s=s, i=i: e.transpose(out=T[:Q, i * 128:i * 128 + P], in_=s, identity=c.identb[:P, :P]),
              reads=[src_tok, c.ident_t], writes=[Tt], merge=(i > 0))
    if halves:
        src3 = T[:, :n * 128].rearrange("p (a b) -> p a b", a=n)
        kb.op("act", lambda e: e.copy(out=dst_ap[0:64, :, 0, :], in_=src3[0:64, :, :]), reads=[Tt], writes=[dst_tok], merge=merge)
        kb.op("dve", lambda e: e.tensor_copy(out=dst_ap[64:128, :, 1, :], in_=src3[64:128, :, :]), reads=[Tt], writes=[dst_tok], merge=True)
        return
    if len(dst_ap.shape) == 3:
        src = T[:Q, :n * 128].rearrange("p (a b) -> p a b", a=n)[:, :, :P]
    else:
        assert P == 128
        src = T[:Q, :n * 128]
    if eng == "act":
        kb.op("act", lambda e: e.copy(out=dst_ap, in_=src), reads=[Tt], writes=[dst_tok], merge=merge)
    else:
        kb.op("dve", lambda e: e.tensor_copy(out=dst_ap, in_=src), reads=[Tt], writes=[dst_tok], merge=merge)


def load_w(c, dst_ap, src_ap, tok, si, merge=False):
    c.kb.dma("pool", dst_ap, src_ap, c.dsem_w[si], writes=[tok], merge=merge)


def xattn(c, l):
    nc, kb, nt = c.nc, c.kb, c.nt
    with ExitStack() as st:
        sb = lambda name, shape, dt: c.sb(name, shape, dt, st)
        g_x, g_x_t = load_bc(c, st, "xa_gx", c.g_xattn[c.li(l)], D)
        g_m, g_m_t = load_bc(c, st, "xa_gm", c.g_mem[c.li(l)], D)
        gq, gq_t = load_bc(c, st, "xa_gq", c.gq_x[c.li(l)], 128)
        gk, gk_t = load_bc(c, st, "xa_gk", c.gk_x[c.li(l)], 128)
        wq = sb("xa_wq", [128, 8, 512], BF16); wq_t = Tok()
        wkv = sb("xa_wkv", [128, 8, 1024], BF16); wkv_t = Tok()
        wo = sb("xa_wo", [128, 4, 1024], BF16); wo_t = Tok()
        load_w(c, wkv[:], c.wkv_x[c.li(l)].rearrange("(k p) n -> p k n", p=128), wkv_t, 0)
        load_w(c, wq[:], c.wq_x[c.li(l)].rearrange("(k p) n -> p k n", p=128), wq_t, 1)
        load_w(c, wo[:], c.wo_x[c.li(l)].rearrange("(k p) n -> p k n", p=128), wo_t, 2)
        KxT = sb("xa_KxT", [128, 4, 256], BF16); KxT_t = Tok()
        Vx = sb("xa_Vx", [128, 2, 4, 129], BF16); Vx_t = Tok()
        scr = sb("xa_scr", [128, D], F32); scr_t = Tok()
        stt = sb("xa_st", [128, 16], F32); stt_t = Tok()
        hb = [sb(f"xa_hb{i}", [128, D], BF16) for i in range(2)]; hb_t = [Tok(), Tok()]
        hT = [sb(f"xa_hT{i}", [128, 8, 128], BF16) for i in range(2)]; hT_t = [Tok(), Tok()]
        mt_in = [sb(f"xa_min{i}", [128, D], F32) for i in range(2)]; mt_in_t = [Tok(), Tok()]
        qf = sb("xa_qf", [128, 512], F32); qf_t = Tok()
        qb = sb("xa_qb", [128, 512], BF16); qb_t = Tok()
        qT = sb("xa_qT", [128, 4, 128], BF16); qT_t = Tok()
        PT = sb("xa_PT", [128, 8, 128], BF16); PT_t = Tok()
        rec = sb("xa_rec", [128, 4], F32); rec_t = Tok()
        ob = sb("xa_ob", [128, 4, 128], BF16); ob_t = Tok()
        oT = sb("xa_oT", [128, 4, 128], BF16); oT_t = Tok()

        kb.flush(c.dsem_misc, c.misc); c.misc = []
        kb.op("dve", lambda e: e.memset(Vx[:, :, :, 128:129], 1.0), writes=[Vx_t])
        for mt in range(2):
            kb.dma("sp", mt_in[mt][:], c.mem[mt * 128:(mt + 1) * 128, :], c.dsem_in[mt], writes=[mt_in_t[mt]])
            rmsnorm_tile(c, mt_in[mt][:], mt_in_t[mt], g_m[:], g_m_t, hb[mt][:], hb_t[mt], D, scr, scr_t, stt, stt_t)
            transposes(c, [hb[mt][:, k * 128:(k + 1) * 128] for k in range(8)], hb_t[mt], hT[mt][:], hT_t[mt])
            for ch in range(2):
                M, Mt = next_M(c)
                for k in range(8):
                    kb.op("pe", lambda e, k=k: e.matmul(M[:], lhsT=hT[mt][:, k, :], rhs=wkv[:, k, ch * 512:(ch + 1) * 512],
                                                        start=(k == 0), stop=(k == 7)),
                          reads=[hT_t[mt], wkv_t], writes=[Mt], merge=(k > 0))
                if ch == 0:
                    kb.op("act", lambda e, M=M: e.copy(out=qf[:], in_=M[:]), reads=[Mt], writes=[qf_t])
                    headnorm(c, qf[:].rearrange("p (h d) -> p h d", h=4), qf_t, 4, 128, gk[:], gk_t,
                             qb[:].rearrange("p (h d) -> p h d", h=4), qb_t, scr, scr_t, stt, stt_t)
                    transposes(c, [qb[:, h * 128:(h + 1) * 128] for h in range(4)], qb_t,
                               KxT[:, :, mt * 128:(mt + 1) * 128], KxT_t, merge=True)
                else:
                    kb.op("act", lambda e: e.copy(out=Vx[:, mt, :, 0:128], in_=M[:].rearrange("p (h d) -> p h d", h=4)),
                          reads=[Mt], writes=[Vx_t], merge=True)
        sc = 128.0 ** -0.5
        scrF = sb("xa_scrF", [128, D], F32); scrF_t = Tok()
        sttF = sb("xa_sttF", [128, 4], F32); sttF_t = Tok()
        qf2 = [qf, sb("xa_qf2", [128, 512], F32)]; qf2_t = [qf_t, Tok()]
        qT2 = [qT, sb("xa_qT2", [128, 4, 128], BF16)]; qT2_t = [qT_t, Tok()]
        PT2 = [PT, sb("xa_PT2", [128, 8, 128], BF16)]; PT2_t = [PT_t, Tok()]

        def xF(t):
            i2 = t % 2
            rmsnorm_tile(c, c.X[:, t, :], c.Xt[t], g_x[:], g_x_t, hb[i2][:], hb_t[i2], D, scrF, scrF_t, sttF, sttF_t)
            transposes(c, [hb[i2][:, k * 128:(k + 1) * 128] for k in range(8)], hb_t[i2], hT[i2][:], hT_t[i2])
            M, Mt = next_M(c)
            for k in range(8):
                kb.op("pe", lambda e, k=k, M=M: e.matmul(M[:], lhsT=hT[i2][:, k, :], rhs=wq[:, k, :], start=(k == 0), stop=(k == 7)),
                      reads=[hT_t[i2], wq_t], writes=[Mt], merge=(k > 0))
            kb.op("act", lambda e, M=M: e.copy(out=qf2[i2][:], in_=M[:]), reads=[Mt], writes=[qf2_t[i2]])

        def xG(t):
            i2 = t % 2
            headnorm(c, qf2[i2][:].rearrange("p (h d) -> p h d", h=4), qf2_t[i2], 4, 128, gq[:], gq_t,
                     qb[:].rearrange("p (h d) -> p h d", h=4), qb_t, scr, scr_t, stt, stt_t)
            transposes(c, [qb[:, h * 128:(h + 1) * 128] for h in range(4)], qb_t, qT2[i2][:], qT2_t[i2])
            Ms = [next_M(c), next_M(c)]
            for h in range(4):
                for mb in range(2):
                    u = h * 2 + mb
                    M, Mt = Ms[u // 4]
                    kb.op("pe", lambda e, h=h, mb=mb, u=u, M=M: e.matmul(
                        M[:, (u % 4) * 128:(u % 4 + 1) * 128], lhsT=KxT[:, h, mb * 128:(mb + 1) * 128], rhs=qT2[i2][:, h, :],
                        start=True, stop=True), reads=[KxT_t, qT2_t[i2]], writes=[Mt], merge=(u % 4 > 0))
            for j in range(2):
                M, Mt = Ms[j]
                kb.op("act", lambda e, j=j, M=M: e.activation(out=PT2[i2][:, j * 4:(j + 1) * 4, :],
                                                             in_=M[:].rearrange("p (a b) -> p a b", a=4),
                                                             func=AF.Exp, scale=sc),
                      reads=[Mt], writes=[PT2_t[i2]], merge=(j > 0))

        def xH(t):
            i2 = t % 2
            O1 = next_M(c); O2 = next_M(c)
            for h in range(4):
                M, Mt = (O1 if h < 3 else O2)
                off = (h % 3) * 129
                for mb in range(2):
                    kb.op("pe", lambda e, h=h, mb=mb, M=M, off=off: e.matmul(
                        M[:, off:off + 129], lhsT=PT2[i2][:, h * 2 + mb, :], rhs=Vx[:, mb, h, :],
                        start=(mb == 0), stop=(mb == 1)), reads=[PT2_t[i2], Vx_t], writes=[Mt], merge=True)
            for h in range(4):
                M, Mt = (O1 if h < 3 else O2)
                off = (h % 3) * 129
                kb.op("dve", lambda e, h=h, M=M, off=off: e.reciprocal(out=rec[:, h:h + 1], in_=M[:, off + 128:off + 129]),
                      reads=[Mt], writes=[rec_t], merge=(h > 0))
            for h in range(4):
                M, Mt = (O1 if h < 3 else O2)
                off = (h % 3) * 129
                kb.op("dve", lambda e, h=h, M=M, off=off: e.tensor_scalar(out=ob[:, h, :], in0=M[:, off:off + 128],
                                                                          scalar1=rec[:, h:h + 1], scalar2=None, op0=ALU.mult),
                      reads=[Mt, rec_t], writes=[ob_t], merge=(h > 0))
            transposes(c, [ob[:, h, :] for h in range(4)], ob_t, oT[:], oT_t)
            for ch in range(2):
                M, Mt = next_M(c)
                for k in range(4):
                    kb.op("pe", lambda e, k=k, M=M: e.matmul(M[:], lhsT=oT[:, k, :], rhs=wo[:, k, ch * 512:(ch + 1) * 512],
                                                             start=(k == 0), stop=(k == 3)),
                          reads=[oT_t, wo_t], writes=[Mt], merge=(k > 0))
                kb.op("dve", lambda e, M=M, ch=ch: e.tensor_tensor(out=c.X[:, t, ch * 512:(ch + 1) * 512], in0=M[:],
                                                                   in1=c.X[:, t, ch * 512:(ch + 1) * 512], op=ALU.add),
                      reads=[Mt, c.Xt[t]], writes=[c.Xt[t]])

        pipeline(nt, [xF, xG, xH])
        kb.barrier()


def mlp(c, l):
    nc, kb, nt = c.nc, c.kb, c.nt
    ntok = nt * 128
    with ExitStack() as st:
        sb = lambda name, shape, dt: c.sb(name, shape, dt, st)
        g, g_t = load_bc(c, st, "ml_g", c.g_mlp[c.li(l)], D)
        scr = sb("ml_scr", [128, D], F32); scr_t = Tok()
        stt = sb("ml_st", [128, 4], F32); stt_t = Tok()
        hb = [sb(f"ml_hb{i}", [128, D], BF16) for i in range(2)]; hb_t = [Tok(), Tok()]
        hT = sb("ml_hT", [128, 8, ntok], BF16); hT_t = [Tok() for _ in range(nt)]
        hid = sb("ml_hid", [128, 4, ntok], BF16)
        wup = [sb(f"ml_wup{i}", [128, 8, 512], BF16) for i in range(2)]; wup_t = [Tok(), Tok()]
        wdn = [sb(f"ml_wdn{i}", [128, 4, 1024], BF16) for i in range(2)]; wdn_t = [Tok(), Tok()]
        rl = [sb(f"ml_rl{i}", [128, 512], F32) for i in range(2)]; rl_t = [Tok(), Tok()]
        wup_v = c.w_up[c.li(l)].rearrange("(k p) n -> p k n", p=128)
        wdn_v = c.w_down[c.li(l)].rearrange("(f p) n -> p f n", p=128)

        def loadg(gi):
            b = gi % 2
            load_w(c, wup[b][:], wup_v[:, :, gi * 512:(gi + 1) * 512], wup_t[b], 0 + b)
            load_w(c, wdn[b][:], wdn_v[:, gi * 4:(gi + 1) * 4, :], wdn_t[b], 2 + b)

        kb.flush(c.dsem_misc, c.misc); c.misc = []
        loadg(0)
        for t in range(nt):
            i2 = t % 2
            rmsnorm_tile(c, c.X[:, t, :], c.Xt[t], g[:], g_t, hb[i2][:], hb_t[i2], D, scr, scr_t, stt, stt_t)
            transposes(c, [hb[i2][:, k * 128:(k + 1) * 128] for k in range(8)], hb_t[i2],
                       hT[:, :, t * 128:(t + 1) * 128], hT_t[t])
        ncks = max(1, ntok // 512)
        hid_t = [Tok() for _ in range(ncks)]
        cw = ntok // ncks
        tiles_per_ck = cw // 128
        ri = 0
        for gi in range(8):
            b = gi % 2
            if gi + 1 < 8:
                loadg(gi + 1)
            for ck in range(ncks):
                for f in range(4):
                    M, Mt = next_M(c)
                    for k in range(8):
                        kb.op("pe", lambda e, k=k, M=M, f=f, ck=ck: e.matmul(
                            M[:, :cw], lhsT=wup[b][:, k, f * 128:(f + 1) * 128], rhs=hT[:, k, ck * cw:(ck + 1) * cw],
                            start=(k == 0), stop=(k == 7)),
                            reads=[wup_t[b]] + hT_t[ck * tiles_per_ck:(ck + 1) * tiles_per_ck], writes=[Mt], merge=(k > 0))
                    r = ri % 2
                    ri += 1
                    kb.op("act", lambda e, M=M, r=r: e.activation(out=rl[r][:, :cw], in_=M[:, :cw], func=AF.Relu),
                          reads=[Mt], writes=[rl_t[r]])
                    kb.op("pool", lambda e, r=r, f=f, ck=ck: e.tensor_tensor(out=hid[:, f, ck * cw:(ck + 1) * cw],
                                                                           in0=rl[r][:, :cw], in1=rl[r][:, :cw], op=ALU.mult),
                          reads=[rl_t[r]], writes=[hid_t[ck]], merge=(f > 0))
            for t in range(nt):
                for ch in range(2):
                    M, Mt = next_M(c)
                    for f in range(4):
                        kb.op("pe", lambda e, f=f, M=M, t=t, ch=ch: e.matmul(
                            M[:], lhsT=hid[:, f, t * 128:(t + 1) * 128], rhs=wdn[b][:, f, ch * 512:(ch + 1) * 512],
                            start=(f == 0), stop=(f == 3)),
                            reads=[hid_t[t // tiles_per_ck], wdn_t[b]], writes=[Mt], merge=(f > 0))
                    kb.op("dve", lambda e, M=M, t=t, ch=ch: e.tensor_tensor(out=c.X[:, t, ch * 512:(ch + 1) * 512], in0=M[:],
                                                                          in1=c.X[:, t, ch * 512:(ch + 1) * 512], op=ALU.add),
                          reads=[Mt, c.Xt[t]], writes=[c.Xt[t]])
        kb.barrier()


def l1_mixer(c, mode):
    nc, kb, nt = c.nc, c.kb, c.nt
    ntok = nt * 128
    nh = nt * 16
    NE = nt * 144
    with ExitStack() as st:
        sb = lambda name, shape, dt: c.sb(name, shape, dt, st)
        g, g_t = load_bc(c, st, "m1_g", c.g_mix[c.li(1)], D)
        scr = sb("m1_scr", [128, D], F32); scr_t = Tok()
        stt = sb("m1_st", [128, 4], F32); stt_t = Tok()
        yT = sb("m1_yT", [128, 8, ntok], BF16); yT_t = [Tok() for _ in range(8)]
        sin_ = ExitStack()
        sb = lambda name, shape, dt: c.sb(name, shape, dt, sin_)
        hb = [sb("m1_hb", [128, D], BF16)] * 2; hb_t = [Tok()] * 2
        hT = sb("m1_hT", [128, 8, ntok], BF16); hT_t = [Tok() for _ in range(nt)]
        nht = max(1, nh // 128)
        hTh = sb("m1_hTh", [128, 8, nh], BF16); hTh_t = Tok()
        xh = [sb("m1_xh", [128, D], F32)] * 2; xh_t = [Tok()] * 2
        if mode == "ALL":
            xhB = sb("m1_xhB", [128, D], F32); xhB_t = Tok()
            hsel = sb("m1_hsel", [128, 2], F32); hsel_t = Tok()
            kb.dma("sp", hsel[:], c.hsel[:, :], c.dsem_misc, writes=[hsel_t])
            c.misc.append(hsel_t)
            tails_t = Tok(); gath_t = Tok()
            ccsem = kb.newsem("cc_sem")
            dsem_t = kb.newsem("d_tails")
            kb.dma("sp", c.tails.ap().rearrange("(t r) d -> r t d", r=16), c.X[112:128, :, :], dsem_t,
                   reads=list(c.Xt), writes=[tails_t])
            kb.collective("AllGather", c.groups, c.tails.ap().opt(), c.gath.ap().opt(), ccsem, reads=[tails_t], writes=[gath_t])
        cw_sb = sb("m1_cw", [128, 4, 3], F32); cw_t = Tok()
        ps_sb = sb("m1_ps", [128, 4, 1], F32); ps_t = Tok()
        invc = sb("m1_invc", [128, 64], F32); invc_t = Tok()
        wpool = sb("m1_wpool", [128, 4, 128], BF16); wpool_t = Tok()
        kb.dma("sp", cw_sb[:].rearrange("p c k -> p (c k)"), c.conv_w[:, :], c.dsem_misc, writes=[cw_t])
        kb.dma("sp", ps_sb[:].rearrange("p c k -> p (c k)"), c.pool_scale[:, :], c.dsem_misc, writes=[ps_t])
        kb.dma("sp", invc[:], c.invc[:, :], c.dsem_misc, writes=[invc_t])
        c.misc += [cw_t, ps_t, invc_t]
        kb.flush(c.dsem_misc, c.misc); c.misc = []
        load_w(c, wpool[:], c.w_pool.rearrange("g c d -> c g d"), wpool_t, 4)
        for ht in range(nht):
            i2 = ht % 2
            rows = min(128, nh)
            if mode == "L1":
                kb.dma("sp", xh[i2][:rows, :], c.xh[ht * 128:ht * 128 + rows, :], c.dsem_in[i2], writes=[xh_t[i2]])
            else:
                gv = c.gath.ap()
                nhr = nt * 16
                kb.dma("sp", xh[i2][:rows, :], gv[ht * 128:ht * 128 + rows, :], c.dsem_in[0], reads=[gath_t], writes=[xh_t[i2]])
                if ht == 0:
                    kb.op("dve", lambda e: e.memset(xhB[:rows, :], 0.0), writes=[xhB_t])
                    kb.dma("sp", xhB[16:rows, :], gv[nhr:nhr + rows - 16, :], c.dsem_in[1], reads=[gath_t], writes=[xhB_t], merge=True)
                else:
                    kb.dma("sp", xhB[:rows, :], gv[nhr + ht * 128 - 16:nhr + ht * 128 - 16 + rows, :], c.dsem_in[1],
                           reads=[gath_t], writes=[xhB_t])
                kb.op("dve", lambda e: e.tensor_scalar(out=xh[i2][:rows, :], in0=xh[i2][:rows, :], scalar1=hsel[:rows, 0:1], scalar2=None,
                                                       op0=ALU.mult), reads=[xh_t[i2], hsel_t], writes=[xh_t[i2]])
                kb.op("dve", lambda e: e.scalar_tensor_tensor(out=xh[i2][:rows, :], in0=xhB[:rows, :], scalar=hsel[:rows, 1:2],
                                                              in1=xh[i2][:rows, :], op0=ALU.mult, op1=ALU.add),
                      reads=[xhB_t, hsel_t, xh_t[i2]], writes=[xh_t[i2]])
            rmsnorm_tile(c, xh[i2][:rows, :], xh_t[i2], g[:rows, :], g_t, hb[i2][:rows, :], hb_t[i2], D,
                         scr[:rows, :], scr_t, stt[:rows, :], stt_t)
            transposes(c, [hb[i2][:rows, k * 128:(k + 1) * 128] for k in range(8)], hb_t[i2],
                       hTh[:, :, ht * 128:ht * 128 + rows], hTh_t, merge=True)
        for t in range(nt):
            i2 = t % 2
            rmsnorm_tile(c, c.X[:, t, :], c.Xt[t], g[:], g_t, hb[i2][:], hb_t[i2], D, scr, scr_t, stt, stt_t)
            transposes(c, [hb[i2][:, k * 128:(k + 1) * 128] for k in range(8)], hb_t[i2],
                       hT[:, :, t * 128:(t + 1) * 128], hT_t[t])
        wv = c.w_in_o.rearrange("(k p) n -> p k n", p=128)
        wch = [sb(f"m1_w{i}", [128, 8, 4, 128], BF16) for i in range(2)]; wch_t = [Tok(), Tok()]

        def loadc(ci):
            b = ci % 2
            for j in range(4):
                load_w(c, wch[b][:, :, j, :], wv[:, :, j * 512 + ci * 128: j * 512 + (ci + 1) * 128], wch_t[b], 0 + b,
                       merge=(j > 0))

        loadc(0)
        ncks = max(1, ntok // 512)
        cwid = ntok // ncks
        bpc = cwid // 128
        U = [sb("m1_U", [128, nt, 144], F32)] * 2; U_t = [Tok()] * 2
        Hc = sb("m1_Hc", [128, nt, 144], F32); Hc_t = Tok()
        Z, Z_t = U[0], U_t[0]
        S1, S1_t = Hc, Hc_t
        S2 = sb("m1_S2", [128, nt, 144], F32); S2_t = Tok()
        cv = sb("m1_cv", [128, nt, 128], F32); cv_t = Tok()
        pl = sb("m1_pl", [128, nt, 128], BF16); pl_t = Tok()

        def proj(j, b, dst, dst_t, own_only=False, evac="act"):
            for ck in range(ncks):
                M, Mt = next_M(c)
                for k in range(8):
                    kb.op("pe", lambda e, k=k, M=M, ck=ck: e.matmul(M[:, :cwid], lhsT=wch[b][:, k, j, :],
                                                                    rhs=hT[:, k, ck * cwid:(ck + 1) * cwid],
                                                                    start=(k == 0), stop=(k == 7)),
                          reads=[wch_t[b]] + hT_t[ck * bpc:(ck + 1) * bpc], writes=[Mt], merge=(k > 0))
                src = M[:, :cwid].rearrange("p (a b) -> p a b", a=bpc)
                if own_only:
                    d = dst[:, ck * bpc:(ck + 1) * bpc, :]
                else:
                    d = dst[:, ck * bpc:(ck + 1) * bpc, 16:144]
                kb.op("act", lambda e, d=d, src=src: e.copy(out=d, in_=src), reads=[Mt], writes=[dst_t], merge=True)
            if not own_only:
                M, Mt = next_M(c)
                for k in range(8):
                    kb.op("pe", lambda e, k=k, M=M: e.matmul(M[:, :nh], lhsT=wch[b][:, k, j, :], rhs=hTh[:, k, :],
                                                             start=(k == 0), stop=(k == 7)),
                          reads=[wch_t[b], hTh_t], writes=[Mt], merge=(k > 0))
                kb.op("act", lambda e, M=M: e.copy(out=dst[:, :, 0:16], in_=M[:, :nh].rearrange("p (a b) -> p a b", a=nt)),
                      reads=[Mt], writes=[dst_t], merge=True)

        wins = [2, 4, 8, 16]
        for ci in range(4):
            b = ci % 2
            if ci + 1 < 4:
                loadc(ci + 1)
            Ub = U[ci % 2]; Ub_t = U_t[ci % 2]
            proj(1, b, Ub, Ub_t)
            proj(2, b, Hc, Hc_t)
            kb.op("dve", lambda e: e.tensor_tensor(out=Ub[:], in0=Ub[:], in1=Hc[:], op=ALU.mult),
                  reads=[Ub_t, Hc_t], writes=[Ub_t])
            kb.op("dve", lambda e: e.tensor_scalar(out=cv[:], in0=Ub[:, :, 16:144], scalar1=cw_sb[:, ci, 2:3], scalar2=None,
                                                   op0=ALU.mult), reads=[Ub_t, cw_t], writes=[cv_t])
            for kk, sh in ((1, 1), (0, 2)):
                kb.op("dve", lambda e, kk=kk, sh=sh: e.scalar_tensor_tensor(
                    out=cv[:], in0=Ub[:, :, 16 - sh:144 - sh], scalar=cw_sb[:, ci, kk:kk + 1], in1=cv[:],
                    op0=ALU.mult, op1=ALU.add), reads=[Ub_t, cw_t, cv_t], writes=[cv_t])
            for ck in range(ncks):
                M, Mt = next_M(c)
                for k in range(8):
                    kb.op("pe", lambda e, k=k, M=M, ck=ck: e.matmul(M[:, :cwid], lhsT=wch[b][:, k, 0, :],
                                                                    rhs=hT[:, k, ck * cwid:(ck + 1) * cwid],
                                                                    start=(k == 0), stop=(k == 7)),
                          reads=[wch_t[b]] + hT_t[ck * bpc:(ck + 1) * bpc], writes=[Mt], merge=(k > 0))
                kb.op("dve", lambda e, M=M, ck=ck: e.tensor_tensor(
                    out=yT[:, ci, ck * cwid:(ck + 1) * cwid], in0=M[:, :cwid],
                    in1=cv[:, ck * bpc:(ck + 1) * bpc, :].rearrange("p a b -> p (a b)"), op=ALU.mult),
                    reads=[Mt, cv_t], writes=[yT_t[ci]], merge=True)
            proj(3, b, Z, Z_t)
            w = wins[ci]
            cur, cur_t = Z, Z_t
            nxt = [(S1, S1_t), (S2, S2_t)]
            sh = 1
            ni = 0
            lo = 0
            while sh < w:
                dstb, dstb_t = nxt[ni % 2]
                ni += 1
                lo2 = lo + sh
                kb.op("dve", lambda e, cur=cur, dstb=dstb, sh=sh, lo2=lo2: e.tensor_tensor(
                    out=dstb[:, :, lo2:144], in0=cur[:, :, lo2:144], in1=cur[:, :, lo2 - sh:144 - sh], op=ALU.add),
                    reads=[cur_t], writes=[dstb_t])
                cur, cur_t = dstb, dstb_t
                lo = lo2
                sh *= 2
            kb.op("dve", lambda e, cur=cur: e.scalar_tensor_tensor(
                out=pl[:], in0=cur[:, :, 16:144], scalar=1.0 / w, in1=Z[:, :, 16:144], op0=ALU.mult, op1=ALU.subtract),
                reads=[cur_t, Z_t], writes=[pl_t])
            kb.op("dve", lambda e, cur=cur: e.tensor_tensor(out=S1[:, 0, 0:16] if cur is not S1 else S2[:, 0, 0:16],
                                                            in0=cur[:, 0, 16:32], in1=invc[:, ci * 16:(ci + 1) * 16], op=ALU.mult),
                  reads=[cur_t, invc_t], writes=[S1_t if cur is not S1 else S2_t])
            fx = S1 if cur is not S1 else S2
            fx_t = S1_t if cur is not S1 else S2_t
            kb.op("dve", lambda e, fx=fx: e.tensor_tensor(out=pl[:, 0, 0:16], in0=fx[:, 0, 0:16], in1=Z[:, 0, 16:32], op=ALU.subtract),
                  reads=[fx_t, Z_t, pl_t], writes=[pl_t])
            for ck in range(ncks):
                M, Mt = next_M(c)
                kb.op("pe", lambda e, M=M, ck=ck: e.matmul(M[:, :cwid], lhsT=wpool[:, ci, :],
                                                           rhs=pl[:, ck * bpc:(ck + 1) * bpc, :].rearrange("p a b -> p (a b)"),
                                                           start=True, stop=True),
                      reads=[wpool_t, pl_t], writes=[Mt])
                kb.op("act", lambda e, M=M, ck=ck: e.activation(out=yT[:, 4 + ci, ck * cwid:(ck + 1) * cwid], in_=M[:, :cwid],
                                                                func=AF.Identity, scale=ps_sb[:, ci, :]),
                      reads=[Mt, ps_t], writes=[yT_t[4 + ci]], merge=True)
        kb.barrier()
        sin_.close()
        sb = lambda name, shape, dt: c.sb(name, shape, dt, st)
        wo = sb("m1_wo", [128, 8, 1024], BF16); wo_t = Tok()
        load_w(c, wo[:], c.w_out_o.rearrange("(k p) n -> p k n", p=128), wo_t, 5)
        for t in range(nt):
            for ch in range(2):
                M, Mt = next_M(c)
                for k in range(8):
                    kb.op("pe", lambda e, k=k, M=M, t=t, ch=ch: e.matmul(
                        M[:], lhsT=yT[:, k, t * 128:(t + 1) * 128], rhs=wo[:, k, ch * 512:(ch + 1) * 512],
                        start=(k == 0), stop=(k == 7)), reads=[yT_t[k], wo_t], writes=[Mt], merge=(k > 0))
                kb.op("dve", lambda e, M=M, t=t, ch=ch: e.tensor_tensor(out=c.X[:, t, ch * 512:(ch + 1) * 512], in0=M[:],
                                                                      in1=c.X[:, t, ch * 512:(ch + 1) * 512], op=ALU.add),
                      reads=[Mt, c.Xt[t]], writes=[c.Xt[t]])
        kb.barrier()


WINS = (2, 4, 8, 16)


def _common_inputs(inp, layer):
    f = lambda a: np.ascontiguousarray(np.asarray(a, dtype=np.float32))
    m = {"ident": np.eye(128, dtype=np.float32)}
    for nm in ["g_mix", "g_xattn", "g_mem", "g_mlp", "wq_x", "wkv_x", "gq_x", "gk_x", "wo_x", "w_up", "w_down"]:
        m[nm] = f(np.asarray(inp[nm])[layer:layer + 1])
    return m


def _l1_inputs(inp, p):
    f = lambda a: np.ascontiguousarray(np.asarray(a, dtype=np.float32))
    invc = np.zeros((128, 64), np.float32)
    for g, w in enumerate(WINS):
        for j in range(16):
            invc[:, g * 16 + j] = 1.0 / (min(j + 1, w) if p == 0 else w)
    return {
        "w_in_o": f(inp["w_in_o"][0]), "conv_w": f(np.asarray(inp["conv_w"][0]).T.reshape(4, 128, 3).transpose(1, 0, 2).reshape(128, 12)),
        "w_pool": f(inp["w_pool"][0]), "pool_scale": f(np.asarray(inp["pool_scale"][0]).reshape(4, 128).T),
        "w_out_o": f(inp["w_out_o"][0]), "invc": invc,
    }


def _own_blocks(a, p, nt):
    blk = a.reshape((2 * nt, 128) + a.shape[1:])
    return np.ascontiguousarray(blk[p::2].reshape((nt * 128,) + a.shape[1:]))


def prep_l1(x1, inp, nt=NT):
    common = _common_inputs(inp, 1)
    maps = []
    for core in range(8):
        b, p = core // 2, core % 2
        m = dict(common)
        m.update(_l1_inputs(inp, p))
        xs = np.asarray(x1[b], dtype=np.float32)
        m["xo"] = _own_blocks(xs, p, nt)
        xh = np.zeros((nt * 16, D), np.float32)
        for i in range(nt):
            g0 = (2 * i + p) * 128
            if g0 >= 16:
                xh[i * 16:(i + 1) * 16] = xs[g0 - 16:g0]
        if nt * 16 < 256:
            xh = np.concatenate([xh, np.zeros((256 - nt * 16, D), np.float32)], 0)
        m["xh"] = xh
        m["mem"] = np.ascontiguousarray(np.asarray(inp["mem"][b], dtype=np.float32))
        maps.append(m)
    return maps


def gather(res, nt=NT, key="out"):
    out = np.zeros((4, 2 * nt * 128, D), np.float32)
    for core in range(8):
        b, p = core // 2, core % 2
        o = np.asarray(res.results[core][key]).reshape(nt, 128, D)
        out[b].reshape(2 * nt, 128, D)[p::2] = o
    return out


def rope_cast(c, T3, T_tok, H, cs_ap, cs_tok, out3, out_tok, tmp, tmp_tok):
    kb = c.kb
    cosb = cs_ap[:, 0:8].unsqueeze(1).broadcast_to([128, H, 8])
    sinb = cs_ap[:, 8:16].unsqueeze(1).broadcast_to([128, H, 8])
    t3 = tmp[:, :H * 32].rearrange("p (h d) -> p h d", h=H)
    x1 = T3[:, :, 0:8]
    x2 = T3[:, :, 8:16]
    kb.op("dve", lambda e: e.tensor_tensor(out=t3[:, :, 0:8], in0=x1, in1=cosb, op=ALU.mult), reads=[T_tok, cs_tok], writes=[tmp_tok])
    kb.op("dve", lambda e: e.tensor_tensor(out=t3[:, :, 8:16], in0=x2, in1=sinb, op=ALU.mult), reads=[T_tok, cs_tok], writes=[tmp_tok], merge=True)
    kb.op("dve", lambda e: e.tensor_tensor(out=t3[:, :, 16:24], in0=x2, in1=cosb, op=ALU.mult), reads=[T_tok, cs_tok], writes=[tmp_tok], merge=True)
    kb.op("dve", lambda e: e.tensor_tensor(out=t3[:, :, 24:32], in0=x1, in1=sinb, op=ALU.mult), reads=[T_tok, cs_tok], writes=[tmp_tok], merge=True)
    kb.op("act", lambda e: e.copy(out=out3[:, :, 16:64], in_=T3[:, :, 16:64]), reads=[T_tok], writes=[out_tok])
    kb.op("dve", lambda e: e.tensor_tensor(out=out3[:, :, 0:8], in0=t3[:, :, 0:8], in1=t3[:, :, 8:16], op=ALU.subtract),
          reads=[tmp_tok], writes=[out_tok], merge=True)
    kb.op("dve", lambda e: e.tensor_tensor(out=out3[:, :, 8:16], in0=t3[:, :, 16:24], in1=t3[:, :, 24:32], op=ALU.add),
          reads=[tmp_tok], writes=[out_tok], merge=True)


def l0_mixer(c, mode):
    nc, kb, nt = c.nc, c.kb, c.nt
    nb = 2 * nt
    ntok = nt * 128
    xo_v = c.xo.rearrange("(t p) d -> p t d", p=128)
    xt_v = c.xt.rearrange("(t p) d -> p t d", p=128)
    with ExitStack() as so:
        sbo = lambda name, shape, dt: c.sb(name, shape, dt, so)
        g, g_t = load_bc(c, so, "m0_g", c.g_mix[c.li(0)], D)
        gqa, gqa_t = load_bc(c, so, "m0_gqa", c.gq_a[0], 64)
        gka, gka_t = load_bc(c, so, "m0_gka", c.gk_a[0], 64)
        gsub, gsub_t = load_bc(c, so, "m0_gsub", c.g_sub_a[0], 128)
        gkv, gkv_t = load_bc(c, so, "m0_gkv", c.g_kv_b[0], 256)
        gqb, gqb_t = load_bc(c, so, "m0_gqb", c.gq_b[0], 64)
        gkb, gkb_t = load_bc(c, so, "m0_gkb", c.gk_b[0], 64)
        gki, gki_t = load_bc(c, so, "m0_gki", c.g_kidx[0], 64)
        lamv = sbo("m0_lamv", [128, 4, 64], F32); lamv_t = Tok()
        kb.dma("sp", lamv[:].rearrange("p a b -> p (a b)"), c.lamv.rearrange("a b -> (a b)").partition_broadcast(128),
               c.dsem_misc, writes=[lamv_t])
        invf = sbo("m0_invf", [128, 16], F32); invf_t = Tok()
        kb.dma("sp", invf[:], c.invf[:, :], c.dsem_misc, writes=[invf_t])
        posi = sbo("m0_posi", [128, nb], I32); posi_t = Tok()
        kb.dma("sp", posi[:], c.pos[:, :], c.dsem_misc, writes=[posi_t])
        msk = sbo("m0_msk", [128, 2, 128], BF16); msk_t = Tok()
        kb.dma("pool", msk[:, 0, :], c.tri[:, :], c.dsem_misc_sw, writes=[msk_t])
        kb.dma("pool", msk[:, 1, :], c.omask[:, :], c.dsem_misc_sw, writes=[msk_t], merge=True)
        negm = sbo("m0_negm", [128, 2, 128], F32); negm_t = Tok()
        kb.dma("sp", negm[:, 0, :], c.trineg[:, :], c.dsem_misc, writes=[negm_t])
        kb.dma("sp", negm[:, 1, :], c.oneg[:, :], c.dsem_misc, writes=[negm_t], merge=True)
        c.misc += [lamv_t, invf_t, posi_t, msk_t, negm_t]
        kb.flush(c.dsem_misc, c.misc); c.misc = []

        lam_init = 0.8 - 0.6 * float(np.exp(-0.3 * 0))
        lsc = sbo("m0_lsc", [128, 8], F32); lsc_t = Tok()
        lpr = sbo("m0_lpr", [128, 2, 64], F32); lpr_t = Tok()
        kb.op("dve", lambda e: e.tensor_tensor(out=lpr[:, 0, :], in0=lamv[:, 0, :], in1=lamv[:, 1, :], op=ALU.mult), reads=[lamv_t], writes=[lpr_t])
        kb.op("dve", lambda e: e.tensor_tensor(out=lpr[:, 1, :], in0=lamv[:, 2, :], in1=lamv[:, 3, :], op=ALU.mult), reads=[lamv_t], writes=[lpr_t], merge=True)
        kb.op("dve", lambda e: e.reduce_sum(out=lsc[:, 0:2], in_=lpr[:], axis=AX.X), reads=[lpr_t], writes=[lsc_t])
        kb.op("act", lambda e: e.activation(out=lsc[:, 2:4], in_=lsc[:, 0:2], func=AF.Exp), reads=[lsc_t], writes=[lsc_t])
        kb.op("dve", lambda e: e.tensor_tensor(out=lsc[:, 4:5], in0=lsc[:, 3:4], in1=lsc[:, 2:3], op=ALU.subtract), reads=[lsc_t], writes=[lsc_t])
        kb.op("dve", lambda e: e.tensor_scalar(out=lsc[:, 5:6], in0=lsc[:, 4:5], scalar1=-lam_init, scalar2=None, op0=ALU.add),
              reads=[lsc_t], writes=[lsc_t])
        nlam = lsc[:, 5:6]
        kb.op("dve", lambda e: e.tensor_scalar(out=gsub[:], in0=gsub[:], scalar1=1.0 - lam_init, scalar2=None, op0=ALU.mult),
              reads=[gsub_t], writes=[gsub_t])

        cs = sbo("m0_cs", [128, nb, 16], F32); cs_t = Tok()
        OAT = sbo("m0_OAT", [128, 4, ntok], BF16); OAT_t = [Tok() for _ in range(nt)]
        stmp = ExitStack()
        posf = c.sb("m0_posf", [128, nb], F32, stmp); posf_t = Tok()
        ang = c.sb("m0_ang", [128, nb, 16], F32, stmp); ang_t = Tok()
        angi = c.sb("m0_angi", [128, nb, 16], I32, stmp); angi_t = Tok()
        kb.op("dve", lambda e: e.tensor_copy(out=posf[:], in_=posi[:]), reads=[posi_t], writes=[posf_t])
        kb.op("dve", lambda e: e.tensor_tensor(out=ang[:], in0=posf[:].unsqueeze(2).broadcast_to([128, nb, 16]),
                                               in1=invf[:].unsqueeze(1).broadcast_to([128, nb, 16]), op=ALU.mult),
              reads=[posf_t, invf_t], writes=[ang_t])
        kb.op("dve", lambda e: e.tensor_scalar(out=ang[:, :, 0:8], in0=ang[:, :, 0:8], scalar1=float(np.pi / 2), scalar2=None, op0=ALU.add),
              reads=[ang_t], writes=[ang_t])
        kb.op("dve", lambda e: e.tensor_scalar(out=angi[:], in0=ang[:], scalar1=float(1.0 / (2 * np.pi)), scalar2=None, op0=ALU.mult),
              reads=[ang_t], writes=[angi_t])
        kb.op("dve", lambda e: e.tensor_copy(out=cs[:], in_=angi[:]), reads=[angi_t], writes=[cs_t])
        kb.op("dve", lambda e: e.scalar_tensor_tensor(out=ang[:], in0=cs[:], scalar=float(-2 * np.pi), in1=ang[:], op0=ALU.mult, op1=ALU.add),
              reads=[cs_t, ang_t], writes=[ang_t])
        kb.op("dve", lambda e: e.tensor_scalar(out=cs[:], in0=ang[:], scalar1=float(np.pi), scalar2=float(-2 * np.pi), op0=ALU.is_gt, op1=ALU.mult),
              reads=[ang_t], writes=[cs_t])
        kb.op("dve", lambda e: e.tensor_tensor(out=ang[:], in0=ang[:], in1=cs[:], op=ALU.add), reads=[ang_t, cs_t], writes=[ang_t])
        kb.op("dve", lambda e: e.tensor_scalar(out=cs[:], in0=ang[:], scalar1=float(-np.pi), scalar2=float(2 * np.pi), op0=ALU.is_lt, op1=ALU.mult),
              reads=[ang_t], writes=[cs_t])
        kb.op("dve", lambda e: e.tensor_tensor(out=ang[:], in0=ang[:], in1=cs[:], op=ALU.add), reads=[ang_t, cs_t], writes=[ang_t])
        kb.op("act", lambda e: e.activation(out=cs[:], in_=ang[:], func=AF.Sin), reads=[ang_t], writes=[cs_t])
        kb.barrier()
        stmp.close()

        scr = sbo("m0_scr", [128, D], F32); scr_t = Tok()
        stt = sbo("m0_st", [128, 64], F32); stt_t = Tok()
        xin = [sbo(f"m0_xin{i}", [128, D], F32) for i in range(2)]; xin_t = [Tok(), Tok()]
        hb = [sbo("m0_hb", [128, D], BF16)] * 2; hb_t = [Tok()] * 2
        hT = [sbo(f"m0_hT{i}", [128, 8, 128], BF16) for i in range(2)]; hT_t = [Tok(), Tok()]
        pf = sbo("m0_pf", [128, 1280], F32); pf_t = Tok()
        pn = sbo("m0_pn", [128, 12, 64], F32); pn_t = Tok()
        pb = sbo("m0_pb", [128, 12, 64], BF16); pb_t = Tok()
        rtmp = sbo("m0_rtmp", [128, 12 * 32], F32); rtmp_t = Tok()
        state = {"n": 0}

        def load_norm_T(src_v, ti, sc_=None):
            i2 = state["n"] % 2
            state["n"] += 1
            sF, sF_t, stF, stF_t = sc_ if sc_ is not None else (scr, scr_t, stt, stt_t)
            kb.dma("sp", xin[i2][:], src_v[:, ti, :], c.dsem_in[i2], writes=[xin_t[i2]])
            rmsnorm_tile(c, xin[i2][:], xin_t[i2], g[:], g_t, hb[i2][:], hb_t[i2], D, sF, sF_t, stF, stF_t)
            transposes(c, [hb[i2][:, k * 128:(k + 1) * 128] for k in range(8)], hb_t[i2], hT[i2][:], hT_t[i2])
            return i2

        def proj_tok(i2, w, w_t, c0, c1, dst_ap, dst_tok, merge=False):
            M, Mt = next_M(c, 0, 4)
            n = c1 - c0
            for k in range(8):
                kb.op("pe", lambda e, k=k: e.matmul(M[:, :n], lhsT=hT[i2][:, k, :], rhs=w[:, k, c0:c1], start=(k == 0), stop=(k == 7)),
                      reads=[hT_t[i2], w_t], writes=[Mt], merge=(k > 0))
            kb.op("act", lambda e: e.copy(out=dst_ap, in_=M[:, :n]), reads=[Mt], writes=[dst_tok], merge=merge)

        import os
        STOP = os.environ.get("L0_STOP", "")
        if STOP == "setup":
            kb.barrier(); return
        with ExitStack() as sa:
            sba = lambda name, shape, dt: c.sb(name, shape, dt, sa)
            KAT = sba("a_KAT", [128, 4, nb * 128], BF16); KAT_t = [Tok() for _ in range(nb)]
            VA = sba("a_VA", [128, nb, 4, 129], BF16); VA_t = [Tok() for _ in range(nb)]
            wk = sba("a_wk", [128, 8, 1024], BF16); wk_t = Tok()
            wq = sba("a_wq", [128, 8, 512], BF16); wq_t = Tok()
            load_w(c, wk[:], c.wA_k.rearrange("(k p) n -> p k n", p=128), wk_t, 0)
            load_w(c, wq[:], c.wA_q.rearrange("(k p) n -> p k n", p=128), wq_t, 1)
            for s in range(nb):
                kb.op("pool", lambda e, s=s: e.memset(VA[:, s, :, 128:129], 1.0), writes=[VA_t[s]])
            with ExitStack() as sak:
                scrF = c.sb("a_scrF", [128, D], F32, sak); scrF_t = Tok()
                sttF = c.sb("a_sttF", [128, 4], F32, sak); sttF_t = Tok()
                pfh_t = [Tok(), Tok()]

                def akF(s):
                    src_v, ti = (xo_v, s // 2) if s % 2 == 0 else (xt_v, s // 2)
                    i2 = load_norm_T(src_v, ti, (scrF, scrF_t, sttF, sttF_t))
                    h2 = s % 2
                    proj_tok(i2, wk, wk_t, 0, 512, pf[:, h2 * 512:(h2 + 1) * 512], pfh_t[h2])
                    M, Mt = next_M(c, 0, 4)
                    for k in range(8):
                        kb.op("pe", lambda e, k=k, M=M: e.matmul(M[:], lhsT=hT[i2][:, k, :], rhs=wk[:, k, 512:1024], start=(k == 0), stop=(k == 7)),
                              reads=[hT_t[i2], wk_t], writes=[Mt], merge=(k > 0))
                    kb.op("act", lambda e, M=M, s=s: e.copy(out=VA[:, s, :, 0:128], in_=M[:].rearrange("p (h d) -> p h d", h=4)),
                          reads=[Mt], writes=[VA_t[s]], merge=True)

                def akB(s):
                    h2 = s % 2
                    headnorm(c, pf[:, h2 * 512:(h2 + 1) * 512].rearrange("p (h d) -> p h d", h=8), pfh_t[h2], 8, 64, gka[:], gka_t,
                             pn[:, 0:8, :], pn_t, scr, scr_t, stt, stt_t)
                    rope_cast(c, pn[:, 0:8, :], pn_t, 8, cs[:, s, :], cs_t, pb[:, 0:8, :], pb_t, rtmp, rtmp_t)
                    pb2 = pb[:, 0:8, :].rearrange("p h d -> p (h d)")
                    transposes(c, [pb2[:, h * 128:(h + 1) * 128] for h in range(4)], pb_t, KAT[:, :, s * 128:(s + 1) * 128], KAT_t[s])

                pipeline(nb, [akF, akB])
                kb.barrier()
            if STOP == "AK":
                kb.barrier(); return
            QAT = [sba(f"a_QAT{i}", [128, 4, 2, 128], BF16) for i in range(2)]; QAT_t = [Tok(), Tok()]
            for i_ in range(2):
                kb.op("pool", lambda e, i_=i_: e.memset(QAT[i_][:], 0.0), writes=[QAT_t[i_]])
            PT = [sba(f"a_PT{i}", [128, 4, 128], BF16) for i in range(3)]; PT_t = [Tok() for _ in range(3)]
            oa2 = [sba(f"a_oa{i}", [128, 4, 128], F32) for i in range(2)]; oa2_t = [Tok(), Tok()]
            oab = sba("a_oab", [128, 4, 128], BF16); oab_t = Tok()
            tq = sba("a_tq", [128, 128], F32); tq_t = Tok()
            rec = sba("a_rec", [128, 8], F32); rec_t = Tok()
            sc = 64.0 ** -0.5
            astate = {"pti": 0}

            def aqF(i):
                i2 = load_norm_T(xo_v, i)
                proj_tok(i2, wq, wq_t, 0, 512, pf[:, 0:512], pf_t)
                headnorm(c, pf[:, 0:512].rearrange("p (h d) -> p h d", h=8), pf_t, 8, 64, gqa[:], gqa_t,
                         pn[:, 0:8, :], pn_t, scr, scr_t, stt, stt_t)
                rope_cast(c, pn[:, 0:8, :], pn_t, 8, cs[:, 2 * i, :], cs_t, pb[:, 0:8, :], pb_t, rtmp, rtmp_t)
                pb2 = pb[:, 0:8, :].rearrange("p h d -> p (h d)")
                qi2 = i % 2
                transposes(c, [pb2[:, h * 128:(h + 1) * 128] for h in range(4)], pb_t, QAT[qi2], QAT_t[qi2], halves=True)

            def aqG(i):
                qi2 = i % 2
                oa, oa_t = oa2[i % 2], oa2_t[i % 2]
                nslots = 2 * i + 2
                for hp in range(2):
                    O1, O1t = c.M[4], c.Mt[4]
                    O2, O2t = c.M[5], c.Mt[5]
                    def emit_SA(s):
                        M, Mt = next_M(c, 0, 4)
                        for u in range(4):
                            h = 2 * hp + u // 2
                            cc = u % 2
                            kb.op("pe", lambda e, M=M, u=u, h=h, cc=cc, s=s: e.matmul(
                                M[:, u * 128:(u + 1) * 128], lhsT=KAT[:, h, s * 128:(s + 1) * 128],
                                rhs=QAT[qi2][:, h, cc, :], start=True, stop=True),
                                reads=[KAT_t[s], QAT_t[qi2]], writes=[Mt], merge=(u > 0))
                        return M, Mt

                    curS = emit_SA(0)
                    for s in range(nslots):
                        M, Mt = curS
                        if s + 1 < nslots:
                            curS = emit_SA(s + 1)
                        p3 = astate["pti"] % 3
                        astate["pti"] += 1
                        kb.op("act", lambda e, M=M, p3=p3: e.activation(out=PT[p3][:], in_=M[:].rearrange("p (a b) -> p a b", a=4),
                                                                     func=AF.Exp, scale=sc), reads=[Mt], writes=[PT_t[p3]])
                        if s >= nslots - 2:
                            mi_ = 0 if s == nslots - 2 else 1
                            kb.op("dve", lambda e, p3=p3, mi_=mi_: e.tensor_tensor(
                                out=PT[p3][:], in0=PT[p3][:], in1=msk[:, mi_, :].unsqueeze(1).broadcast_to([128, 4, 128]), op=ALU.mult),
                                reads=[PT_t[p3], msk_t], writes=[PT_t[p3]])
                        for u in range(4):
                            h = 2 * hp + u // 2
                            O, Ot = (O1, O1t) if u < 3 else (O2, O2t)
                            off = (u % 3) * 129
                            kb.op("pe", lambda e, O=O, off=off, u=u, h=h, s=s, p3=p3: e.matmul(
                                O[:, off:off + 129], lhsT=PT[p3][:, u, :], rhs=VA[:, s, h, :], start=(s == 0 and u in (0, 3)),
                                stop=(s == nslots - 1), skip_group_check=True),
                                reads=[PT_t[p3], VA_t[s]], writes=[Ot], merge=True)
                    for u in range(4):
                        O, Ot = (O1, O1t) if u < 3 else (O2, O2t)
                        off = (u % 3) * 129
                        kb.op("dve", lambda e, O=O, off=off, u=u: e.reciprocal(out=rec[:, u:u + 1], in_=O[:, off + 128:off + 129]),
                              reads=[Ot], writes=[rec_t], merge=(u > 0))
                    kb.op("dve", lambda e: e.tensor_scalar(out=rec[:, 4:8], in0=rec[:, 0:4], scalar1=nlam, scalar2=None, op0=ALU.mult),
                          reads=[rec_t, lsc_t], writes=[rec_t])
                    for hh in range(2):
                        h = 2 * hp + hh
                        u0, u1 = 2 * hh, 2 * hh + 1
                        Oa, Oat = (O1, O1t) if u0 < 3 else (O2, O2t)
                        Ob, Obt = (O1, O1t) if u1 < 3 else (O2, O2t)
                        offa, offb = (u0 % 3) * 129, (u1 % 3) * 129
                        kb.op("dve", lambda e, Ob=Ob, offb=offb, u1=u1: e.tensor_scalar(out=tq[:], in0=Ob[:, offb:offb + 128],
                                                                                      scalar1=rec[:, 4 + u1:5 + u1], scalar2=None, op0=ALU.mult),
                              reads=[Obt, rec_t], writes=[tq_t])
                        kb.op("dve", lambda e, Oa=Oa, offa=offa, u0=u0, h=h: e.scalar_tensor_tensor(
                            out=oa[:, h, :], in0=Oa[:, offa:offa + 128], scalar=rec[:, u0:u0 + 1], in1=tq[:], op0=ALU.mult, op1=ALU.add),
                            reads=[Oat, rec_t, tq_t], writes=[oa_t], merge=True)

            def aqH(i):
                oa, oa_t = oa2[i % 2], oa2_t[i % 2]
                headnorm(c, oa[:], oa_t, 4, 128, gsub[:], gsub_t, oab[:], oab_t, scr, scr_t, stt, stt_t)
                transposes(c, [oab[:, h, :] for h in range(4)], oab_t, OAT[:, :, i * 128:(i + 1) * 128], OAT_t[i])

            pipeline(nt, [aqF, aqG, aqH])
            kb.barrier()

        if STOP == "A":
            kb.barrier(); return
        with ExitStack() as sB:
            sbb = lambda name, shape, dt: c.sb(name, shape, dt, sB)
            KBT = sbb("b_KBT", [128, 4, nb * 128], BF16); KBT_t = [Tok() for _ in range(nb)]
            VB = sbb("b_VB", [128, nb, 8, 65], BF16); VB_t = [Tok() for _ in range(nb)]
            KIT = sbb("b_KIT", [128, nb * 128], BF16); KIT_t = [Tok() for _ in range(nb)]
            for s in range(nb):
                kb.op("pool", lambda e, s=s: e.memset(VB[:, s, :, 64:65], 1.0), writes=[VB_t[s]])
            with ExitStack() as sk:
                sbk = lambda name, shape, dt: c.sb(name, shape, dt, sk)
                wk = sbk("b_wk", [128, 8, 320], BF16); wk_t = Tok()
                wup = sbk("b_wup", [128, 2, 1024], BF16); wup_t = Tok()
                load_w(c, wk[:], c.wB_k.rearrange("(k p) n -> p k n", p=128), wk_t, 0)
                load_w(c, wup[:], c.w_kv_up.rearrange("(k p) n -> p k n", p=128), wup_t, 1)
                cb = sbk("b_cb", [128, 256], BF16); cb_t = Tok()
                cT = sbk("b_cT", [128, 2, 128], BF16); cT_t = Tok()
                scrF = sbk("b_scrF", [128, D], F32); scrF_t = Tok()
                sttF = sbk("b_sttF", [128, 4], F32); sttF_t = Tok()
                pfa_t = [Tok(), Tok()]
                pfk_t = Tok()

                def bkF(s):
                    src_v, ti = (xo_v, s // 2) if s % 2 == 0 else (xt_v, s // 2)
                    i2 = load_norm_T(src_v, ti, (scrF, scrF_t, sttF, sttF_t))
                    h2 = s % 2
                    proj_tok(i2, wk, wk_t, 0, 320, pf[:, h2 * 320:(h2 + 1) * 320], pfa_t[h2])

                def bkB(s):
                    h2 = s % 2
                    o = h2 * 320
                    rmsnorm_tile(c, pf[:, o:o + 256], pfa_t[h2], gkv[:], gkv_t, cb[:], cb_t, 256, scr, scr_t, stt, stt_t)
                    transposes(c, [cb[:, k * 128:(k + 1) * 128] for k in range(2)], cb_t, cT[:], cT_t)
                    headnorm(c, pf[:, o + 256:o + 320].rearrange("p (h d) -> p h d", h=1), pfa_t[h2], 1, 64, gki[:], gki_t,
                             pn[:, 8:9, :], pn_t, scr, scr_t, stt, stt_t)
                    M, Mt = next_M(c, 0, 4)
                    for k in range(2):
                        kb.op("pe", lambda e, k=k, M=M: e.matmul(M[:], lhsT=cT[:, k, :], rhs=wup[:, k, 0:512], start=(k == 0), stop=(k == 1)),
                              reads=[cT_t, wup_t], writes=[Mt], merge=(k > 0))
                    kb.op("act", lambda e, M=M: e.copy(out=pf[:, 768:1280], in_=M[:]), reads=[Mt], writes=[pfk_t])
                    M2, M2t = next_M(c, 0, 4)
                    for k in range(2):
                        kb.op("pe", lambda e, k=k, M2=M2: e.matmul(M2[:], lhsT=cT[:, k, :], rhs=wup[:, k, 512:1024], start=(k == 0), stop=(k == 1)),
                              reads=[cT_t, wup_t], writes=[M2t], merge=(k > 0))
                    kb.op("act", lambda e, M2=M2, s=s: e.copy(out=VB[:, s, :, 0:64], in_=M2[:].rearrange("p (h d) -> p h d", h=8)),
                          reads=[M2t], writes=[VB_t[s]], merge=True)
                    headnorm(c, pf[:, 768:1280].rearrange("p (h d) -> p h d", h=8), pfk_t, 8, 64, gkb[:], gkb_t,
                             pn[:, 0:8, :], pn_t, scr, scr_t, stt, stt_t)
                    rope_cast(c, pn[:, 0:9, :], pn_t, 9, cs[:, s, :], cs_t, pb[:, 0:9, :], pb_t, rtmp, rtmp_t)
                    kb.op("act", lambda e: e.copy(out=pb[:, 9, :], in_=pb[:, 8, :]), reads=[pb_t], writes=[pb_t])
                    pb2 = pb[:, 0:10, :].rearrange("p h d -> p (h d)")
                    transposes(c, [pb2[:, h * 128:(h + 1) * 128] for h in range(4)], pb_t, KBT[:, :, s * 128:(s + 1) * 128], KBT_t[s])
                    transposes(c, [pb2[:, 512:640]], pb_t, KIT[:, s * 128:(s + 1) * 128], KIT_t[s])

                pipeline(nb, [bkF, bkB])
                kb.barrier()
            if STOP == "BK":
                kb.barrier(); return
            with ExitStack() as sq:
                sbq = lambda name, shape, dt: c.sb(name, shape, dt, sq)
                wq = sbq("b_wq", [128, 8, 772], BF16); wq_t = Tok()
                wo = sbq("b_wo", [128, 8, 1024], BF16); wo_t = Tok()
                load_w(c, wq[:], c.wB_q.rearrange("(k p) n -> p k n", p=128), wq_t, 0)
                load_w(c, wo[:], c.w_out_e.rearrange("(k p) n -> p k n", p=128), wo_t, 1)
                QBT2 = [sbq(f"b_QBT{i}", [128, 4, 2, 128], BF16) for i in range(2)]; QBT2_t = [Tok(), Tok()]
                QIT = sbq("b_QIT", [128, 2, 2, 128], BF16); QIT_t = Tok()
                for i_ in range(2):
                    kb.op("pool", lambda e, i_=i_: e.memset(QBT2[i_][:], 0.0), writes=[QBT2_t[i_]])
                kb.op("pool", lambda e: e.memset(QIT[:], 0.0), writes=[QIT_t])
                wsc = sbq("b_wsc", [128, 4], F32); wsc_t = Tok()
                isc = sbq("b_isc", [128, nb * 128], F32); isc_t = Tok()
                mkb = sbq("b_mkb", [128, nb * 128], BF16); mkb_t = Tok()
                mkT2 = [sbq("b_mkT", [128, nb * 128], BF16)] * 2; mkT2_t = [Tok()] * 2
                Rr = [sbq(f"b_R{i}", [128, 512], F32) for i in range(2)]; Rr_t = [Tok(), Tok()]
                PT8 = [sbq(f"b_PT{i}", [128, 8, 128], BF16) for i in range(2)]; PT8_t = [Tok(), Tok()]
                bs = sbq("b_bs", [128, 16], F32); bs_t = Tok()
                Rk = sbq("b_Rk", [128, BIS_ITERS + 1], F32); Rk_t = Tok()
                pw2 = sbq("b_pw2", [128, BIS_ITERS + 1], F32); pw2_t = Tok()
                for k_ in range(BIS_ITERS + 1):
                    kb.op("pool", lambda e, k_=k_: e.memset(pw2[:, k_:k_ + 1], float(2.0 ** -k_)), writes=[pw2_t], merge=True)
                recb = sbq("b_recb", [128, 8, 1], F32); recb_t = Tok()
                ob = sbq("b_ob", [128, 8, 64], BF16); ob_t = Tok()
                OBT = sbq("b_OBT", [128, 4, 128], BF16); OBT_t = Tok()
                sc = 64.0 ** -0.5
                bst = {"ri": 0, "pi8": 0, "i2": {}}

                def bqF(i):
                    i2 = load_norm_T(xo_v, i)
                    bst["i2"][i] = i2
                    QBT, QBT_t = QBT2[i % 2], QBT2_t[i % 2]
                    mkT, mkT_t = mkT2[i % 2], mkT2_t[i % 2]
                    proj_tok(i2, wq, wq_t, 0, 512, pf[:, 0:512], pf_t)
                    proj_tok(i2, wq, wq_t, 512, 772, pf[:, 512:772], pf_t, merge=True)
                    headnorm(c, pf[:, 0:512].rearrange("p (h d) -> p h d", h=8), pf_t, 8, 64, gqb[:], gqb_t,
                             pn[:, 0:8, :], pn_t, scr, scr_t, stt, stt_t)
                    kb.op("act", lambda e: e.copy(out=pn[:, 8:12, :], in_=pf[:, 512:768].rearrange("p (h d) -> p h d", h=4)),
                          reads=[pf_t], writes=[pn_t], merge=True)
                    kb.op("dve", lambda e: e.tensor_scalar(out=wsc[:], in0=pf[:, 768:772], scalar1=1.0 / 16.0, scalar2=None, op0=ALU.mult),
                          reads=[pf_t], writes=[wsc_t])
                    rope_cast(c, pn[:, 0:12, :], pn_t, 12, cs[:, 2 * i, :], cs_t, pb[:, 0:12, :], pb_t, rtmp, rtmp_t)
                    pb2 = pb[:, 0:12, :].rearrange("p h d -> p (h d)")
                    transposes(c, [pb2[:, h * 128:(h + 1) * 128] for h in range(4)], pb_t, QBT, QBT_t, halves=True)
                    transposes(c, [pb2[:, 512 + h * 128:512 + (h + 1) * 128] for h in range(2)], pb_t, QIT, QIT_t, halves=True)
                    nslots = 2 * i + 2
                    nk = nslots * 128
                    for c0 in range(0, nk, 512):
                        wd = min(512, nk - c0)
                        sl = list(range(c0 // 128, (c0 + wd) // 128))
                        for h in range(4):
                            M, Mt = next_M(c, 0, 4)
                            pr = (h % 2) * 64
                            kb.op("pe", lambda e, M=M, h=h, pr=pr, c0=c0, wd=wd: e.matmul(
                                M[:, :wd], lhsT=QIT[:, h // 2, h % 2, :], rhs=KIT[:, c0:c0 + wd], start=True, stop=True),
                                reads=[QIT_t] + [KIT_t[s] for s in sl], writes=[Mt])
                            r = bst["ri"] % 2
                            bst["ri"] += 1
                            kb.op("act", lambda e, M=M, r=r, wd=wd: e.activation(out=Rr[r][:, :wd], in_=M[:, :wd], func=AF.Relu),
                                  reads=[Mt], writes=[Rr_t[r]])
                            if h == 0:
                                kb.op("dve", lambda e, r=r, c0=c0, wd=wd: e.tensor_scalar(out=isc[:, c0:c0 + wd], in0=Rr[r][:, :wd],
                                                                                        scalar1=wsc[:, 0:1], scalar2=None, op0=ALU.mult),
                                      reads=[Rr_t[r], wsc_t], writes=[isc_t], merge=True)
                            else:
                                kb.op("dve", lambda e, r=r, c0=c0, wd=wd, h=h: e.scalar_tensor_tensor(
                                    out=isc[:, c0:c0 + wd], in0=Rr[r][:, :wd], scalar=wsc[:, h:h + 1], in1=isc[:, c0:c0 + wd],
                                    op0=ALU.mult, op1=ALU.add), reads=[Rr_t[r], wsc_t, isc_t], writes=[isc_t], merge=True)
                    kb.op("dve", lambda e: e.reduce_max(out=bs[:, 0:1], in_=isc[:, :nk], axis=AX.X, apply_absolute_value=True),
                          reads=[isc_t], writes=[bs_t])
                    kb.op("dve", lambda e: e.tensor_scalar(out=bs[:, 1:2], in0=bs[:, 0:1], scalar1=1.001, scalar2=1e-3, op0=ALU.mult, op1=ALU.add),
                          reads=[bs_t], writes=[bs_t])
                    kb.op("dve", lambda e: e.tensor_scalar(out=Rk[:], in0=pw2[:], scalar1=bs[:, 1:2], scalar2=None, op0=ALU.mult),
                          reads=[bs_t, pw2_t], writes=[Rk_t])
                    kb.op("dve", lambda e: e.memset(bs[:, 2:3], 0.0), writes=[bs_t], merge=True)
                    for mi_, s in ((0, nslots - 2), (1, nslots - 1)):
                        kb.op("dve", lambda e, mi_=mi_, s=s: e.tensor_tensor(out=isc[:, s * 128:(s + 1) * 128], in0=isc[:, s * 128:(s + 1) * 128],
                                                                         in1=negm[:, mi_, :], op=ALU.add),
                              reads=[isc_t, negm_t], writes=[isc_t])
                    mid, cnt, tp_, thr = bs[:, 2:3], bs[:, 3:4], bs[:, 4:5], bs[:, 5:6]
                    for it in range(BIS_ITERS):
                        kb.op("dve", lambda e: e.tensor_scalar(out=mkb[:, :nk], in0=isc[:, :nk], scalar1=mid, scalar2=None,
                                                               op0=ALU.is_gt, op1=ALU.add, accum_out=cnt),
                              reads=[isc_t, bs_t], writes=[mkb_t, bs_t])
                        kb.op("dve", lambda e: e.tensor_scalar(out=tp_, in0=cnt, scalar1=float(c.topk) - 0.5, scalar2=0.5,
                                                               op0=ALU.is_ge, op1=ALU.subtract), reads=[bs_t], writes=[bs_t])
                        kb.op("dve", lambda e, it=it: e.scalar_tensor_tensor(out=mid, in0=tp_, scalar=Rk[:, it:it + 1], in1=mid,
                                                                             op0=ALU.mult, op1=ALU.add), reads=[bs_t, Rk_t], writes=[bs_t])
                    kb.op("dve", lambda e: e.scalar_tensor_tensor(out=thr, in0=Rk[:, BIS_ITERS:BIS_ITERS + 1], scalar=-1.0, in1=mid,
                                                                  op0=ALU.mult, op1=ALU.add), reads=[bs_t, Rk_t], writes=[bs_t])
                    kb.op("dve", lambda e: e.tensor_scalar(out=mkb[:, :nk], in0=isc[:, :nk], scalar1=thr, scalar2=None, op0=ALU.is_gt),
                          reads=[isc_t, bs_t], writes=[mkb_t])

                def bqH(i):
                    mkT, mkT_t = mkT2[i % 2], mkT2_t[i % 2]
                    nslots = 2 * i + 2
                    for s0 in range(0, nslots, 8):
                        n8 = min(8, nslots - s0)
                        transposes(c, [mkb[:, (s0 + j) * 128:(s0 + j + 1) * 128] for j in range(n8)], mkb_t,
                                   mkT[:, s0 * 128:(s0 + n8) * 128], mkT_t, eng="act", merge=True)

                def bqG(i):
                    i2 = bst["i2"][i]
                    QBT, QBT_t = QBT2[i % 2], QBT2_t[i % 2]
                    mkT, mkT_t = mkT2[i % 2], mkT2_t[i % 2]
                    nslots = 2 * i + 2
                    O1, O1t = c.M[4], c.Mt[4]
                    O2, O2t = c.M[5], c.Mt[5]
                    def emit_SB(s):
                        Ms = [next_M(c, 0, 4), next_M(c, 0, 4)]
                        for u in range(8):
                            M, Mt = Ms[u // 4]
                            kb.op("pe", lambda e, M=M, u=u, s=s: e.matmul(
                                M[:, (u % 4) * 128:(u % 4 + 1) * 128], lhsT=KBT[:, u // 2, s * 128:(s + 1) * 128],
                                rhs=QBT[:, u // 2, u % 2, :], start=True, stop=True),
                                reads=[KBT_t[s], QBT_t], writes=[Mt], merge=(u % 4 > 0))
                        return Ms

                    curS = emit_SB(0)
                    for s in range(nslots):
                        Ms = curS
                        if s + 1 < nslots:
                            curS = emit_SB(s + 1)
                        p8 = bst["pi8"] % 2
                        bst["pi8"] += 1
                        for j in range(2):
                            M, Mt = Ms[j]
                            kb.op("act", lambda e, M=M, j=j, p8=p8: e.activation(out=PT8[p8][:, j * 4:(j + 1) * 4, :],
                                                                             in_=M[:].rearrange("p (a b) -> p a b", a=4), func=AF.Exp, scale=sc),
                                  reads=[Mt], writes=[PT8_t[p8]], merge=(j > 0))
                        kb.op(MASK_ENG, lambda e, p8=p8, s=s: e.tensor_tensor(
                            out=PT8[p8][:], in0=PT8[p8][:], in1=mkT[:, s * 128:(s + 1) * 128].unsqueeze(1).broadcast_to([128, 8, 128]), op=ALU.mult),
                            reads=[PT8_t[p8], mkT_t], writes=[PT8_t[p8]])
                        for u in range(8):
                            O, Ot = (O1, O1t) if u < 4 else (O2, O2t)
                            off = (u % 4) * 65
                            kb.op("pe", lambda e, O=O, off=off, u=u, s=s, p8=p8: e.matmul(
                                O[:, off:off + 65], lhsT=PT8[p8][:, u, :], rhs=VB[:, s, u, :], start=(s == 0 and u in (0, 4)),
                                stop=(s == nslots - 1), skip_group_check=True),
                                reads=[PT8_t[p8], VB_t[s]], writes=[Ot], merge=True)
                    for j, (O, Ot) in enumerate(((O1, O1t), (O2, O2t))):
                        O3 = O[:, 0:260].rearrange("p (u d) -> p u d", u=4)
                        kb.op("dve", lambda e, O3=O3, j=j: e.reciprocal(out=recb[:, j * 4:(j + 1) * 4, :], in_=O3[:, :, 64:65]),
                              reads=[Ot], writes=[recb_t], merge=(j > 0))
                    for j, (O, Ot) in enumerate(((O1, O1t), (O2, O2t))):
                        O3 = O[:, 0:260].rearrange("p (u d) -> p u d", u=4)
                        kb.op("dve", lambda e, O3=O3, j=j: e.tensor_tensor(out=ob[:, j * 4:(j + 1) * 4, :], in0=O3[:, :, 0:64],
                                                                        in1=recb[:, j * 4:(j + 1) * 4, :].broadcast_to([128, 4, 64]), op=ALU.mult),
                              reads=[Ot, recb_t], writes=[ob_t], merge=(j > 0))
                    ob2 = ob[:].rearrange("p h d -> p (h d)")
                    transposes(c, [ob2[:, h * 128:(h + 1) * 128] for h in range(4)], ob_t, OBT[:], OBT_t)
                    xi = i % 2
                    for ch in range(2):
                        M, Mt = next_M(c, 0, 4)
                        for k in range(8):
                            if k < 4:
                                kb.op("pe", lambda e, M=M, k=k, ch=ch: e.matmul(M[:], lhsT=OAT[:, k, i * 128:(i + 1) * 128],
                                                                                rhs=wo[:, k, ch * 512:(ch + 1) * 512], start=(k == 0), stop=False),
                                      reads=[OAT_t[i], wo_t], writes=[Mt], merge=(k > 0))
                            else:
                                kb.op("pe", lambda e, M=M, k=k, ch=ch: e.matmul(M[:], lhsT=OBT[:, k - 4, :],
                                                                                rhs=wo[:, k, ch * 512:(ch + 1) * 512], start=False, stop=(k == 7)),
                                      reads=[OBT_t, wo_t], writes=[Mt], merge=True)
                        kb.op("dve", lambda e, M=M, ch=ch: e.tensor_tensor(out=xin[i2][:, ch * 512:(ch + 1) * 512], in0=M[:],
                                                                         in1=xin[i2][:, ch * 512:(ch + 1) * 512], op=ALU.add),
                              reads=[Mt, xin_t[i2]], writes=[xin_t[i2]])
                    kb.dma("sp", c.x1s_v[:, i, :], xin[i2][:], c.dsem_x1[xi], reads=[xin_t[i2]], writes=[c.x1s_t[i]])

                bqF(0)
                for i in range(nt):
                    bqH(i)
                    if i + 1 < nt:
                        bqF(i + 1)
                    bqG(i)
            kb.barrier()


def _l0_inputs(inp, p):
    f = lambda a: np.ascontiguousarray(np.asarray(a, dtype=np.float32))
    w = np.asarray(inp["w_in_e"][0], dtype=np.float32)
    qa, ka, va, qb, ckv, qi, ki, wi = np.split(w, np.cumsum([512, 512, 512, 512, 256, 256, 64, 4])[:-1], axis=1)
    k = np.arange(128)
    tri = (k[:, None] <= k[None, :]).astype(np.float32)
    inv = (500000.0 ** (-np.arange(0, 16, 2, dtype=np.float32) / 16)).astype(np.float32)
    return {
        "invf": np.tile(np.concatenate([inv, inv])[None, :], (128, 1)).astype(np.float32),
        "tri": tri, "omask": np.full((128, 128), float(p), np.float32),
        "trineg": np.where(tri.T > 0, 0.0, NEG).astype(np.float32),
        "oneg": np.full((128, 128), 0.0 if p == 1 else NEG, np.float32),
        "lamv": f(np.stack([inp["lam_q1"][0], inp["lam_k1"][0], inp["lam_q2"][0], inp["lam_k2"][0]])),
        "gq_a": f(inp["gq_a"]), "gk_a": f(inp["gk_a"]), "g_sub_a": f(inp["g_sub_a"]), "g_kv_b": f(inp["g_kv_b"]),
        "gq_b": f(inp["gq_b"]), "gk_b": f(inp["gk_b"]), "g_kidx": f(inp["g_kidx"]),
        "wA_k": f(np.concatenate([ka, va], 1)), "wA_q": f(qa),
        "wB_k": f(np.concatenate([ckv, ki], 1)), "wB_q": f(np.concatenate([qb, qi, wi], 1)),
        "w_kv_up": f(inp["w_kv_up_b"][0]), "w_out_e": f(inp["w_out_e"][0]),
    }


def prep_l0(inp, nt=NT, mixonly=False, layers=(0,)):
    f = lambda a: np.ascontiguousarray(np.asarray(a, dtype=np.float32))
    maps = []
    x = np.asarray(inp["x"], dtype=np.float32)
    pos = np.asarray(inp["positions"]).astype(np.int32)
    ls = list(layers)
    for core in range(8):
        b, p = core // 2, core % 2
        m = {"ident": np.eye(128, dtype=np.float32), "g_mix": f(np.asarray(inp["g_mix"])[ls])}
        if not mixonly:
            for nm in ["g_xattn", "g_mem", "g_mlp", "wq_x", "wkv_x", "gq_x", "gk_x", "wo_x", "w_up", "w_down"]:
                m[nm] = f(np.asarray(inp[nm])[ls])
            m["mem"] = f(inp["mem"][b])
        m.update(_l0_inputs(inp, p))
        m["xo"] = _own_blocks(x[b], p, nt)
        m["xt"] = _own_blocks(x[b], 1 - p, nt)
        pb = pos[b].reshape(2 * nt, 128)
        slots = np.empty((2 * nt, 128), np.int32)
        slots[0::2] = pb[p::2]
        slots[1::2] = pb[(1 - p)::2]
        m["pos"] = np.ascontiguousarray(slots.T)
        maps.append(m)
    return maps


def prep_all(inp, nt=NT):
    maps = prep_l0(inp, nt, mixonly=False, layers=(0, 1))
    for core in range(8):
        p = core % 2
        maps[core].update(_l1_inputs(inp, p))
        hs = np.zeros((128, 2), np.float32)
        hs[:, 0] = 1.0 if p == 1 else 0.0
        hs[:, 1] = 1.0 if p == 0 else 0.0
        maps[core]["hsel"] = hs
    return maps


def kernel(**inputs):
    inp = {k: np.asarray(v) for k, v in inputs.items()}
    nc, _ = build("ALL", NT)
    res = run_bass_kernel_spmd(nc, prep_all(inp, NT), core_ids=list(range(8)))
    return gather(res, NT)
```

```python
import numpy as np
from contextlib import ExitStack
import concourse.bass as bass
import concourse.mybir as mybir
from concourse.bass_utils import run_bass_kernel_spmd

F32 = mybir.dt.float32
BF16 = mybir.dt.bfloat16
I32 = mybir.dt.int32
AF = mybir.ActivationFunctionType
ALU = mybir.AluOpType
AX = mybir.AxisListType

D = 1024
NT = 16
NB = 32
EPS = 1e-6
NEG = -1.0e30
BIS_ITERS = 15
MASK_ENG = "pool"
TOPK = 256


class Sem:
    def __init__(self, h, name):
        self.h = h
        self.cnt = 0
        self.name = name


class Tok:
    __slots__ = ("w", "r")

    def __init__(self):
        self.w = {}
        self.r = {}


def _mx(d, s):
    for k, v in s.items():
        if d.get(k, 0) < v:
            d[k] = v


class KB:
    def __init__(self, nc):
        self.nc = nc
        self.es = ExitStack()
        self.engs = {"pe": nc.tensor, "act": nc.scalar, "dve": nc.vector,
                     "pool": nc.gpsimd, "sp": nc.sync}
        self.allsems = []
        self.esem = {k: self.newsem("s_" + k) for k in self.engs}
        self.waited = {k: {} for k in self.engs}
        self.nins = 0

    def newsem(self, name):
        h = self.es.enter_context(self.nc.semaphore(name))
        s = Sem(h, name)
        self.allsems.append(s)
        return s

    def _wait(self, e, deps):
        w = self.waited[e]
        for s, v in deps.items():
            if e == "pe" and s is self.esem["pe"]:
                continue
            if w.get(s, 0) >= v:
                continue
            self.engs[e].wait_ge(s.h, v)
            w[s] = v
            self.nins += 1

    def _deps(self, reads, writes):
        deps = {}
        for t in reads:
            _mx(deps, t.w)
        for t in writes:
            _mx(deps, t.w)
            _mx(deps, t.r)
        return deps

    def _mark(self, s, reads, writes, merge):
        for t in reads:
            t.r[s] = s.cnt
        for t in writes:
            if merge:
                t.w[s] = s.cnt
            else:
                t.w = {s: s.cnt}
                t.r = {}

    def op(self, e, fn, reads=(), writes=(), merge=False):
        self._wait(e, self._deps(reads, writes))
        ins = fn(self.engs[e])
        s = self.esem[e]
        s.cnt += 1
        ins.then_inc(s.h, 1)
        self.nins += 1
        self._mark(s, reads, writes, merge)

    def dma(self, q, out, in_, dsem, reads=(), writes=(), merge=False):
        self._wait(q, self._deps(reads, writes))
        ins = self.engs[q].dma_start(out=out, in_=in_)
        dsem.cnt += 16
        ins.then_inc(dsem.h, 16)
        self.nins += 1
        self._mark(dsem, reads, writes, merge)

    def collective(self, kind, groups, in_ap, out_ap, csem, reads=(), writes=()):
        self._wait("pool", self._deps(reads, writes))
        ins = self.nc.gpsimd.collective_compute(kind, ALU.bypass, replica_groups=groups, ins=[in_ap], outs=[out_ap])
        csem.cnt += 1
        ins.then_inc(csem.h)
        self.nins += 1
        self._mark(csem, reads, writes, False)

    def flush(self, dsem, toks):
        for t in toks:
            for d in list(t.w):
                if d is dsem or d.name.startswith("d_misc"):
                    t.w[d] = d.cnt

    def barrier(self):
        for e in self.engs:
            deps = {s: s.cnt for s in self.allsems if s.cnt > 0}
            self._wait(e, deps)


class Ctx:
    pass


def build(mode="ALL", nt=NT, topk=TOPK, groups=None):
    nc = bass.Bass("TRN2", target_bir_lowering=False)
    kb = KB(nc)
    c = Ctx()
    c.nc, c.kb, c.nt, c.topk = nc, kb, nt, topk
    c.groups = groups if groups is not None else [[0, 1], [2, 3], [4, 5], [6, 7]]
    ntok = nt * 128
    nl = 2 if mode == "ALL" else 1
    c.li = (lambda l: l) if mode == "ALL" else (lambda l: 0)
    has0 = mode in ("L0mix", "L0", "ALL")
    has1 = mode in ("L1", "ALL")

    def din(name, shape, dt=F32):
        return nc.dram_tensor(name, list(shape), dt, kind="ExternalInput").ap()

    c.xo = din("xo", [ntok, D])
    c.ident = din("ident", [128, 128])
    c.g_mix = din("g_mix", [nl, D])
    if mode != "L0mix":
        c.mem = din("mem", [256, D])
        c.g_xattn = din("g_xattn", [nl, D])
        c.g_mem = din("g_mem", [nl, D])
        c.g_mlp = din("g_mlp", [nl, D])
        c.wq_x = din("wq_x", [nl, D, 512])
        c.wkv_x = din("wkv_x", [nl, D, 1024])
        c.gq_x = din("gq_x", [nl, 128])
        c.gk_x = din("gk_x", [nl, 128])
        c.wo_x = din("wo_x", [nl, 512, D])
        c.w_up = din("w_up", [nl, D, 4096])
        c.w_down = din("w_down", [nl, 4096, D])
    if has0:
        c.xt = din("xt", [ntok, D])
        c.pos = din("pos", [128, 2 * nt], I32)
        c.invf = din("invf", [128, 16])
        c.tri = din("tri", [128, 128])
        c.omask = din("omask", [128, 128])
        c.trineg = din("trineg", [128, 128])
        c.oneg = din("oneg", [128, 128])
        c.lamv = din("lamv", [4, 64])
        c.gq_a = din("gq_a", [1, 64]); c.gk_a = din("gk_a", [1, 64]); c.g_sub_a = din("g_sub_a", [1, 128])
        c.g_kv_b = din("g_kv_b", [1, 256]); c.gq_b = din("gq_b", [1, 64]); c.gk_b = din("gk_b", [1, 64])
        c.g_kidx = din("g_kidx", [1, 64])
        c.wA_k = din("wA_k", [D, 1024]); c.wA_q = din("wA_q", [D, 512])
        c.wB_k = din("wB_k", [D, 320]); c.wB_q = din("wB_q", [D, 772])
        c.w_kv_up = din("w_kv_up", [256, 1024]); c.w_out_e = din("w_out_e", [D, D])
    if has1:
        c.w_in_o = din("w_in_o", [D, 2048])
        c.conv_w = din("conv_w", [128, 12])
        c.w_pool = din("w_pool", [4, 128, 128])
        c.pool_scale = din("pool_scale", [128, 4])
        c.w_out_o = din("w_out_o", [D, D])
        c.invc = din("invc", [128, 64])
    if mode == "L1":
        c.xh = din("xh", [256, D])
    if mode == "ALL":
        c.hsel = din("hsel", [128, 2])
        c.tails = nc.dram_tensor("tails", [nt * 16, D], F32)
        c.gath = nc.dram_tensor("gath", [2 * nt * 16, D], F32)
    c.out = nc.dram_tensor("out", [ntok, D], F32, kind="ExternalOutput").ap()
    out_v = c.out.rearrange("(t p) d -> p t d", p=128)
    if mode == "L0mix":
        c.x1s_v = out_v
    elif has0:
        c.x1s = nc.dram_tensor("x1s", [ntok, D], F32).ap()
        c.x1s_v = c.x1s.rearrange("(t p) d -> p t d", p=128)
    c.x1s_t = [Tok() for _ in range(nt)]

    es = kb.es
    with es:
        c.uid = 0

        def sb(name, shape, dt, stack=es):
            c.uid += 1
            return stack.enter_context(nc.sbuf_tensor(f"{name}_{c.uid}", list(shape), dt))

        c.sb = sb
        c.T = []
        c.Tt = []
        for i in range(2):
            c.T.append(es.enter_context(nc.psum_tensor(f"psT{i}", [128, 1024], BF16)))
            c.Tt.append(Tok())
        c.M = []
        c.Mt = []
        for i in range(6):
            c.M.append(es.enter_context(nc.psum_tensor(f"psM{i}", [128, 512], F32)))
            c.Mt.append(Tok())
        c.ti = 0
        c.mi = 0
        c.identb = sb("identb", [128, 128], BF16)
        c.ident_t = Tok()
        c.dsem_misc = kb.newsem("d_misc")
        c.dsem_misc_sw = kb.newsem("d_misc_sw")
        c.misc = [c.ident_t]
        c.dsem_in = [kb.newsem("d_in0"), kb.newsem("d_in1")]
        c.dsem_x1 = [kb.newsem("d_x1a"), kb.newsem("d_x1b")]
        kb.dma("pool", c.identb[:], c.ident[:, :], c.dsem_misc_sw, writes=[c.ident_t])
        c.neghalf = sb("neghalf", [128, 16], F32)
        c.neghalf_t = Tok()
        kb.op("pool", lambda e: e.memset(c.neghalf[:], -0.5), writes=[c.neghalf_t])
        c.dsem_x = kb.newsem("d_x")
        c.dsem_w = [kb.newsem(f"d_w{i}") for i in range(8)]
        c.dsem_out = kb.newsem("d_out")

        if has0:
            l0_mixer(c, mode)
        if mode != "L0mix":
            c.X = sb("X", [128, nt, D], F32)
            c.Xt = [Tok() for _ in range(nt)]
            if mode == "L1":
                xo_v = c.xo.rearrange("(t p) d -> p t d", p=128)
                for t in range(nt):
                    kb.dma("sp", c.X[:, t, :], xo_v[:, t, :], c.dsem_x, writes=[c.Xt[t]])
            else:
                for t in range(nt):
                    kb.dma("sp", c.X[:, t, :], c.x1s_v[:, t, :], c.dsem_x, reads=[c.x1s_t[t]], writes=[c.Xt[t]])
            kb.flush(c.dsem_x, c.Xt)
            if has0:
                xattn(c, 0)
                mlp(c, 0)
            if has1:
                l1_mixer(c, mode)
                xattn(c, 1)
                mlp(c, 1)
            for t in range(nt):
                kb.dma("sp", out_v[:, t, :], c.X[:, t, :], c.dsem_out, reads=[c.Xt[t]])
        kb.barrier()
    c.nins = kb.nins
    return nc, c


def pipeline(n, stages):
    k = len(stages)
    for step in range(n + k - 1):
        for si in range(k):
            t = step - si
            if 0 <= t < n:
                stages[si](t)


def next_T(c):
    i = c.ti
    c.ti = (c.ti + 1) % 2
    return c.T[i], c.Tt[i]


def next_M(c, lo=0, hi=6):
    if c.mi < lo or c.mi >= hi:
        c.mi = lo
    i = c.mi
    c.mi = c.mi + 1
    if c.mi >= hi:
        c.mi = lo
    return c.M[i], c.Mt[i]


def load_bc(c, stack, name, src_row, n, q="sp"):
    t = c.sb(name, [128, n], F32, stack)
    tok = Tok()
    c.kb.dma(q, t[:], src_row.partition_broadcast(128), c.dsem_misc, writes=[tok])
    c.misc.append(tok)
    return t, tok


def rmsnorm_tile(c, x_ap, x_tok, g_ap, g_tok, out_ap, out_tok, n, scr, scr_tok, st, st_tok):
    kb = c.kb
    kb.op("dve", lambda e: e.tensor_tensor(out=scr[:, :n], in0=x_ap, in1=x_ap, op=ALU.mult),
          reads=[x_tok], writes=[scr_tok])
    kb.op("dve", lambda e: e.reduce_sum(out=st[:, 0:1], in_=scr[:, :n], axis=AX.X),
          reads=[scr_tok], writes=[st_tok])
    kb.op("dve", lambda e: e.tensor_scalar(out=st[:, 1:2], in0=st[:, 0:1], scalar1=1.0 / n, scalar2=EPS,
                                           op0=ALU.mult, op1=ALU.add),
          reads=[st_tok], writes=[st_tok])
    npar = x_ap.shape[0]
    kb.op("pool", lambda e: e.tensor_tensor(out=st[:, 3:4], in0=st[:, 1:2], in1=c.neghalf[:npar, 0:1], op=ALU.pow),
          reads=[st_tok, c.neghalf_t], writes=[st_tok])
    kb.op("dve", lambda e: e.scalar_tensor_tensor(out=out_ap, in0=x_ap, scalar=st[:, 3:4], in1=g_ap,
                                                  op0=ALU.mult, op1=ALU.mult),
          reads=[x_tok, st_tok, g_tok], writes=[out_tok])


def headnorm(c, src_ap, src_tok, H, Dh, g_ap, g_tok, out_ap, out_tok, scr, scr_tok, st, st_tok, extra_scale=1.0):
    kb = c.kb
    n = H * Dh
    scr3 = scr[:, :n].rearrange("p (h d) -> p h d", h=H)
    kb.op("dve", lambda e: e.tensor_tensor(out=scr3, in0=src_ap, in1=src_ap, op=ALU.mult),
          reads=[src_tok], writes=[scr_tok])
    kb.op("dve", lambda e: e.reduce_sum(out=st[:, 0:H], in_=scr3, axis=AX.X),
          reads=[scr_tok], writes=[st_tok])
    kb.op("dve", lambda e: e.tensor_scalar(out=st[:, H:2 * H], in0=st[:, 0:H], scalar1=1.0 / Dh, scalar2=EPS,
                                           op0=ALU.mult, op1=ALU.add),
          reads=[st_tok], writes=[st_tok])
    kb.op("pool", lambda e: e.tensor_tensor(out=st[:, 3 * H:4 * H], in0=st[:, H:2 * H], in1=c.neghalf[:, 0:H], op=ALU.pow),
          reads=[st_tok, c.neghalf_t], writes=[st_tok])
    rb = st[:, 3 * H:4 * H].unsqueeze(2).broadcast_to([128, H, Dh])
    kb.op("dve", lambda e: e.tensor_tensor(out=scr3, in0=src_ap, in1=rb, op=ALU.mult),
          reads=[src_tok, st_tok], writes=[scr_tok])
    gb = g_ap.unsqueeze(1).broadcast_to([128, H, Dh])
    if extra_scale == 1.0:
        kb.op("dve", lambda e: e.tensor_tensor(out=out_ap, in0=scr3, in1=gb, op=ALU.mult),
              reads=[scr_tok, g_tok], writes=[out_tok])
    else:
        kb.op("dve", lambda e: e.scalar_tensor_tensor(out=out_ap, in0=scr3, scalar=float(extra_scale), in1=gb,
                                                      op0=ALU.mult, op1=ALU.mult),
              reads=[scr_tok, g_tok], writes=[out_tok])


def transposes(c, srcs, src_tok, dst_ap, dst_tok, eng="act", merge=False, halves=False):
    kb = c.kb
    T, Tt = next_T(c)
    n = len(srcs)
    P = srcs[0].shape[0]
    Q = srcs[0].shape[1]
    for i, s in enumerate(srcs):
        kb.op("pe", lambda e, s=s, i=i: e.transpose(out=T[:Q, i * 128:i * 128 + P], in_=s, identity=c.identb[:P, :P]),
              reads=[src_tok, c.ident_t], writes=[Tt], merge=(i > 0))
    if halves:
        src3 = T[:, :n * 128].rearrange("p (a b) -> p a b", a=n)
        kb.op("act", lambda e: e.copy(out=dst_ap[0:64, :, 0, :], in_=src3[0:64, :, :]), reads=[Tt], writes=[dst_tok], merge=merge)
        kb.op("dve", lambda e: e.tensor_copy(out=dst_ap[64:128, :, 1, :], in_=src3[64:128, :, :]), reads=[Tt], writes=[dst_tok], merge=True)
        return
    if len(dst_ap.shape) == 3:
        src = T[:Q, :n * 128].rearrange("p (a b) -> p a b", a=n)[:, :, :P]
    else:
        assert P == 128
        src = T[:Q, :n * 128]
    if eng == "act":
        kb.op("act", lambda e: e.copy(out=dst_ap, in_=src), reads=[Tt], writes=[dst_tok], merge=merge)
    else:
        kb.op("dve", lambda e: e.tensor_copy(out=dst_ap, in_=src), reads=[Tt], writes=[dst_tok], merge=merge)


def load_w(c, dst_ap, src_ap, tok, si, merge=False):
    c.kb.dma("pool", dst_ap, src_ap, c.dsem_w[si], writes=[tok], merge=merge)


def xattn(c, l):
    nc, kb, nt = c.nc, c.kb, c.nt
    with ExitStack() as st:
        sb = lambda name, shape, dt: c.sb(name, shape, dt, st)
        g_x, g_x_t = load_bc(c, st, "xa_gx", c.g_xattn[c.li(l)], D)
        g_m, g_m_t = load_bc(c, st, "xa_gm", c.g_mem[c.li(l)], D)
        gq, gq_t = load_bc(c, st, "xa_gq", c.gq_x[c.li(l)], 128)
        gk, gk_t = load_bc(c, st, "xa_gk", c.gk_x[c.li(l)], 128)
        wq = sb("xa_wq", [128, 8, 512], BF16); wq_t = Tok()
        wkv = sb("xa_wkv", [128, 8, 1024], BF16); wkv_t = Tok()
        wo = sb("xa_wo", [128, 4, 1024], BF16); wo_t = Tok()
        load_w(c, wkv[:], c.wkv_x[c.li(l)].rearrange("(k p) n -> p k n", p=128), wkv_t, 0)
        load_w(c, wq[:], c.wq_x[c.li(l)].rearrange("(k p) n -> p k n", p=128), wq_t, 1)
        load_w(c, wo[:], c.wo_x[c.li(l)].rearrange("(k p) n -> p k n", p=128), wo_t, 2)
        KxT = sb("xa_KxT", [128, 4, 256], BF16); KxT_t = Tok()
        Vx = sb("xa_Vx", [128, 2, 4, 129], BF16); Vx_t = Tok()
        scr = sb("xa_scr", [128, D], F32); scr_t = Tok()
        stt = sb("xa_st", [128, 16], F32); stt_t = Tok()
        hb = [sb(f"xa_hb{i}", [128, D], BF16) for i in range(2)]; hb_t = [Tok(), Tok()]
        hT = [sb(f"xa_hT{i}", [128, 8, 128], BF16) for i in range(2)]; hT_t = [Tok(), Tok()]
        mt_in = [sb(f"xa_min{i}", [128, D], F32) for i in range(2)]; mt_in_t = [Tok(), Tok()]
        qf = sb("xa_qf", [128, 512], F32); qf_t = Tok()
        qb = sb("xa_qb", [128, 512], BF16); qb_t = Tok()
        qT = sb("xa_qT", [128, 4, 128], BF16); qT_t = Tok()
        PT = sb("xa_PT", [128, 8, 128], BF16); PT_t = Tok()
        rec = sb("xa_rec", [128, 4], F32); rec_t = Tok()
        ob = sb("xa_ob", [128, 4, 128], BF16); ob_t = Tok()
        oT = sb("xa_oT", [128, 4, 128], BF16); oT_t = Tok()

        kb.flush(c.dsem_misc, c.misc); c.misc = []
        kb.op("dve", lambda e: e.memset(Vx[:, :, :, 128:129], 1.0), writes=[Vx_t])
        for mt in range(2):
            kb.dma("sp", mt_in[mt][:], c.mem[mt * 128:(mt + 1) * 128, :], c.dsem_in[mt], writes=[mt_in_t[mt]])
            rmsnorm_tile(c, mt_in[mt][:], mt_in_t[mt], g_m[:], g_m_t, hb[mt][:], hb_t[mt], D, scr, scr_t, stt, stt_t)
            transposes(c, [hb[mt][:, k * 128:(k + 1) * 128] for k in range(8)], hb_t[mt], hT[mt][:], hT_t[mt])
            for ch in range(2):
                M, Mt = next_M(c)
                for k in range(8):
                    kb.op("pe", lambda e, k=k: e.matmul(M[:], lhsT=hT[mt][:, k, :], rhs=wkv[:, k, ch * 512:(ch + 1) * 512],
                                                        start=(k == 0), stop=(k == 7)),
                          reads=[hT_t[mt], wkv_t], writes=[Mt], merge=(k > 0))
                if ch == 0:
                    kb.op("act", lambda e, M=M: e.copy(out=qf[:], in_=M[:]), reads=[Mt], writes=[qf_t])
                    headnorm(c, qf[:].rearrange("p (h d) -> p h d", h=4), qf_t, 4, 128, gk[:], gk_t,
                             qb[:].rearrange("p (h d) -> p h d", h=4), qb_t, scr, scr_t, stt, stt_t)
                    transposes(c, [qb[:, h * 128:(h + 1) * 128] for h in range(4)], qb_t,
                               KxT[:, :, mt * 128:(mt + 1) * 128], KxT_t, merge=True)
                else:
                    kb.op("act", lambda e: e.copy(out=Vx[:, mt, :, 0:128], in_=M[:].rearrange("p (h d) -> p h d", h=4)),
                          reads=[Mt], writes=[Vx_t], merge=True)
        sc = 128.0 ** -0.5
        scrF = sb("xa_scrF", [128, D], F32); scrF_t = Tok()
        sttF = sb("xa_sttF", [128, 4], F32); sttF_t = Tok()
        qf2 = [qf, sb("xa_qf2", [128, 512], F32)]; qf2_t = [qf_t, Tok()]
        qT2 = [qT, sb("xa_qT2", [128, 4, 128], BF16)]; qT2_t = [qT_t, Tok()]
        PT2 = [PT, sb("xa_PT2", [128, 8, 128], BF16)]; PT2_t = [PT_t, Tok()]

        def xF(t):
            i2 = t % 2
            rmsnorm_tile(c, c.X[:, t, :], c.Xt[t], g_x[:], g_x_t, hb[i2][:], hb_t[i2], D, scrF, scrF_t, sttF, sttF_t)
            transposes(c, [hb[i2][:, k * 128:(k + 1) * 128] for k in range(8)], hb_t[i2], hT[i2][:], hT_t[i2])
            M, Mt = next_M(c)
            for k in range(8):
                kb.op("pe", lambda e, k=k, M=M: e.matmul(M[:], lhsT=hT[i2][:, k, :], rhs=wq[:, k, :], start=(k == 0), stop=(k == 7)),
                      reads=[hT_t[i2], wq_t], writes=[Mt], merge=(k > 0))
            kb.op("act", lambda e, M=M: e.copy(out=qf2[i2][:], in_=M[:]), reads=[Mt], writes=[qf2_t[i2]])

        def xG(t):
            i2 = t % 2
            headnorm(c, qf2[i2][:].rearrange("p (h d) -> p h d", h=4), qf2_t[i2], 4, 128, gq[:], gq_t,
                     qb[:].rearrange("p (h d) -> p h d", h=4), qb_t, scr, scr_t, stt, stt_t)
            transposes(c, [qb[:, h * 128:(h + 1) * 128] for h in range(4)], qb_t, qT2[i2][:], qT2_t[i2])
            Ms = [next_M(c), next_M(c)]
            for h in range(4):
                for mb in range(2):
                    u = h * 2 + mb
                    M, Mt = Ms[u // 4]
                    kb.op("pe", lambda e, h=h, mb=mb, u=u, M=M: e.matmul(
                        M[:, (u % 4) * 128:(u % 4 + 1) * 128], lhsT=KxT[:, h, mb * 128:(mb + 1) * 128], rhs=qT2[i2][:, h, :],
                        start=True, stop=True), reads=[KxT_t, qT2_t[i2]], writes=[Mt], merge=(u % 4 > 0))
            for j in range(2):
                M, Mt = Ms[j]
                kb.op("act", lambda e, j=j, M=M: e.activation(out=PT2[i2][:, j * 4:(j + 1) * 4, :],
                                                             in_=M[:].rearrange("p (a b) -> p a b", a=4),
                                                             func=AF.Exp, scale=sc),
                      reads=[Mt], writes=[PT2_t[i2]], merge=(j > 0))

        def xH(t):
            i2 = t % 2
            O1 = next_M(c); O2 = next_M(c)
            for h in range(4):
                M, Mt = (O1 if h < 3 else O2)
                off = (h % 3) * 129
                for mb in range(2):
                    kb.op("pe", lambda e, h=h, mb=mb, M=M, off=off: e.matmul(
                        M[:, off:off + 129], lhsT=PT2[i2][:, h * 2 + mb, :], rhs=Vx[:, mb, h, :],
                        start=(mb == 0), stop=(mb == 1)), reads=[PT2_t[i2], Vx_t], writes=[Mt], merge=True)
            for h in range(4):
                M, Mt = (O1 if h < 3 else O2)
                off = (h % 3) * 129
                kb.op("dve", lambda e, h=h, M=M, off=off: e.reciprocal(out=rec[:, h:h + 1], in_=M[:, off + 128:off + 129]),
                      reads=[Mt], writes=[rec_t], merge=(h > 0))
            for h in range(4):
                M, Mt = (O1 if h < 3 else O2)
                off = (h % 3) * 129
                kb.op("dve", lambda e, h=h, M=M, off=off: e.tensor_scalar(out=ob[:, h, :], in0=M[:, off:off + 128],
                                                                          scalar1=rec[:, h:h + 1], scalar2=None, op0=ALU.mult),
                      reads=[Mt, rec_t], writes=[ob_t], merge=(h > 0))
            transposes(c, [ob[:, h, :] for h in range(4)], ob_t, oT[:], oT_t)
            for ch in range(2):
                M, Mt = next_M(c)
                for k in range(4):
                    kb.op("pe", lambda e, k=k, M=M: e.matmul(M[:], lhsT=oT[:, k, :], rhs=wo[:, k, ch * 512:(ch + 1) * 512],
                                                             start=(k == 0), stop=(k == 3)),
                          reads=[oT_t, wo_t], writes=[Mt], merge=(k > 0))
                kb.op("dve", lambda e, M=M, ch=ch: e.tensor_tensor(out=c.X[:, t, ch * 512:(ch + 1) * 512], in0=M[:],
                                                                   in1=c.X[:, t, ch * 512:(ch + 1) * 512], op=ALU.add),
                      reads=[Mt, c.Xt[t]], writes=[c.Xt[t]])

        pipeline(nt, [xF, xG, xH])
        kb.barrier()


def mlp(c, l):
    nc, kb, nt = c.nc, c.kb, c.nt
    ntok = nt * 128
    with ExitStack() as st:
        sb = lambda name, shape, dt: c.sb(name, shape, dt, st)
        g, g_t = load_bc(c, st, "ml_g", c.g_mlp[c.li(l)], D)
        scr = sb("ml_scr", [128, D], F32); scr_t = Tok()
        stt = sb("ml_st", [128, 4], F32); stt_t = Tok()
        hb = [sb(f"ml_hb{i}", [128, D], BF16) for i in range(2)]; hb_t = [Tok(), Tok()]
        hT = sb("ml_hT", [128, 8, ntok], BF16); hT_t = [Tok() for _ in range(nt)]
        hid = sb("ml_hid", [128, 4, ntok], BF16)
        wup = [sb(f"ml_wup{i}", [128, 8, 512], BF16) for i in range(2)]; wup_t = [Tok(), Tok()]
        wdn = [sb(f"ml_wdn{i}", [128, 4, 1024], BF16) for i in range(2)]; wdn_t = [Tok(), Tok()]
        rl = [sb(f"ml_rl{i}", [128, 512], F32) for i in range(2)]; rl_t = [Tok(), Tok()]
        wup_v = c.w_up[c.li(l)].rearrange("(k p) n -> p k n", p=128)
        wdn_v = c.w_down[c.li(l)].rearrange("(f p) n -> p f n", p=128)

        def loadg(gi):
            b = gi % 2
            load_w(c, wup[b][:], wup_v[:, :, gi * 512:(gi + 1) * 512], wup_t[b], 0 + b)
            load_w(c, wdn[b][:], wdn_v[:, gi * 4:(gi + 1) * 4, :], wdn_t[b], 2 + b)

        kb.flush(c.dsem_misc, c.misc); c.misc = []
        loadg(0)
        for t in range(nt):
            i2 = t % 2
            rmsnorm_tile(c, c.X[:, t, :], c.Xt[t], g[:], g_t, hb[i2][:], hb_t[i2], D, scr, scr_t, stt, stt_t)
            transposes(c, [hb[i2][:, k * 128:(k + 1) * 128] for k in range(8)], hb_t[i2],
                       hT[:, :, t * 128:(t + 1) * 128], hT_t[t])
        ncks = max(1, ntok // 512)
        hid_t = [Tok() for _ in range(ncks)]
        cw = ntok // ncks
        tiles_per_ck = cw // 128
        ri = 0
        for gi in range(8):
            b = gi % 2
            if gi + 1 < 8:
                loadg(gi + 1)
            for ck in range(ncks):
                for f in range(4):
                    M, Mt = next_M(c)
                    for k in range(8):
                        kb.op("pe", lambda e, k=k, M=M, f=f, ck=ck: e.matmul(
                            M[:, :cw], lhsT=wup[b][:, k, f * 128:(f + 1) * 128], rhs=hT[:, k, ck * cw:(ck + 1) * cw],
                            start=(k == 0), stop=(k == 7)),
                            reads=[wup_t[b]] + hT_t[ck * tiles_per_ck:(ck + 1) * tiles_per_ck], writes=[Mt], merge=(k > 0))
                    r = ri % 2
                    ri += 1
                    kb.op("act", lambda e, M=M, r=r: e.activation(out=rl[r][:, :cw], in_=M[:, :cw], func=AF.Relu),
                          reads=[Mt], writes=[rl_t[r]])
                    kb.op("pool", lambda e, r=r, f=f, ck=ck: e.tensor_tensor(out=hid[:, f, ck * cw:(ck + 1) * cw],
                                                                           in0=rl[r][:, :cw], in1=rl[r][:, :cw], op=ALU.mult),
                          reads=[rl_t[r]], writes=[hid_t[ck]], merge=(f > 0))
            for t in range(nt):
                for ch in range(2):
                    M, Mt = next_M(c)
                    for f in range(4):
                        kb.op("pe", lambda e, f=f, M=M, t=t, ch=ch: e.matmul(
                            M[:], lhsT=hid[:, f, t * 128:(t + 1) * 128], rhs=wdn[b][:, f, ch * 512:(ch + 1) * 512],
                            start=(f == 0), stop=(f == 3)),
                            reads=[hid_t[t // tiles_per_ck], wdn_t[b]], writes=[Mt], merge=(f > 0))
                    kb.op("dve", lambda e, M=M, t=t, ch=ch: e.tensor_tensor(out=c.X[:, t, ch * 512:(ch + 1) * 512], in0=M[:],
                                                                          in1=c.X[:, t, ch * 512:(ch + 1) * 512], op=ALU.add),
                          reads=[Mt, c.Xt[t]], writes=[c.Xt[t]])
        kb.barrier()


def l1_mixer(c, mode):
    nc, kb, nt = c.nc, c.kb, c.nt
    ntok = nt * 128
    nh = nt * 16
    NE = nt * 144
    with ExitStack() as st:
        sb = lambda name, shape, dt: c.sb(name, shape, dt, st)
        g, g_t = load_bc(c, st, "m1_g", c.g_mix[c.li(1)], D)
        scr = sb("m1_scr", [128, D], F32); scr_t = Tok()
        stt = sb("m1_st", [128, 4], F32); stt_t = Tok()
        yT = sb("m1_yT", [128, 8, ntok], BF16); yT_t = [Tok() for _ in range(8)]
        sin_ = ExitStack()
        sb = lambda name, shape, dt: c.sb(name, shape, dt, sin_)
        hb = [sb("m1_hb", [128, D], BF16)] * 2; hb_t = [Tok()] * 2
        hT = sb("m1_hT", [128, 8, ntok], BF16); hT_t = [Tok() for _ in range(nt)]
        nht = max(1, nh // 128)
        hTh = sb("m1_hTh", [128, 8, nh], BF16); hTh_t = Tok()
        xh = [sb("m1_xh", [128, D], F32)] * 2; xh_t = [Tok()] * 2
        if mode == "ALL":
            xhB = sb("m1_xhB", [128, D], F32); xhB_t = Tok()
            hsel = sb("m1_hsel", [128, 2], F32); hsel_t = Tok()
            kb.dma("sp", hsel[:], c.hsel[:, :], c.dsem_misc, writes=[hsel_t])
            c.misc.append(hsel_t)
            tails_t = Tok(); gath_t = Tok()
            ccsem = kb.newsem("cc_sem")
            dsem_t = kb.newsem("d_tails")
            kb.dma("sp", c.tails.ap().rearrange("(t r) d -> r t d", r=16), c.X[112:128, :, :], dsem_t,
                   reads=list(c.Xt), writes=[tails_t])
            kb.collective("AllGather", c.groups, c.tails.ap().opt(), c.gath.ap().opt(), ccsem, reads=[tails_t], writes=[gath_t])
        cw_sb = sb("m1_cw", [128, 4, 3], F32); cw_t = Tok()
        ps_sb = sb("m1_ps", [128, 4, 1], F32); ps_t = Tok()
        invc = sb("m1_invc", [128, 64], F32); invc_t = Tok()
        wpool = sb("m1_wpool", [128, 4, 128], BF16); wpool_t = Tok()
        kb.dma("sp", cw_sb[:].rearrange("p c k -> p (c k)"), c.conv_w[:, :], c.dsem_misc, writes=[cw_t])
        kb.dma("sp", ps_sb[:].rearrange("p c k -> p (c k)"), c.pool_scale[:, :], c.dsem_misc, writes=[ps_t])
        kb.dma("sp", invc[:], c.invc[:, :], c.dsem_misc, writes=[invc_t])
        c.misc += [cw_t, ps_t, invc_t]
        kb.flush(c.dsem_misc, c.misc); c.misc = []
        load_w(c, wpool[:], c.w_pool.rearrange("g c d -> c g d"), wpool_t, 4)
        for ht in range(nht):
            i2 = ht % 2
            rows = min(128, nh)
            if mode == "L1":
                kb.dma("sp", xh[i2][:rows, :], c.xh[ht * 128:ht * 128 + rows, :], c.dsem_in[i2], writes=[xh_t[i2]])
            else:
                gv = c.gath.ap()
                nhr = nt * 16
                kb.dma("sp", xh[i2][:rows, :], gv[ht * 128:ht * 128 + rows, :], c.dsem_in[0], reads=[gath_t], writes=[xh_t[i2]])
                if ht == 0:
                    kb.op("dve", lambda e: e.memset(xhB[:rows, :], 0.0), writes=[xhB_t])
                    kb.dma("sp", xhB[16:rows, :], gv[nhr:nhr + rows - 16, :], c.dsem_in[1], reads=[gath_t], writes=[xhB_t], merge=True)
                else:
                    kb.dma("sp", xhB[:rows, :], gv[nhr + ht * 128 - 16:nhr + ht * 128 - 16 + rows, :], c.dsem_in[1],
                           reads=[gath_t], writes=[xhB_t])
                kb.op("dve", lambda e: e.tensor_scalar(out=xh[i2][:rows, :], in0=xh[i2][:rows, :], scalar1=hsel[:rows, 0:1], scalar2=None,
                                                       op0=ALU.mult), reads=[xh_t[i2], hsel_t], writes=[xh_t[i2]])
                kb.op("dve", lambda e: e.scalar_tensor_tensor(out=xh[i2][:rows, :], in0=xhB[:rows, :], scalar=hsel[:rows, 1:2],
                                                              in1=xh[i2][:rows, :], op0=ALU.mult, op1=ALU.add),
                      reads=[xhB_t, hsel_t, xh_t[i2]], writes=[xh_t[i2]])
            rmsnorm_tile(c, xh[i2][:rows, :], xh_t[i2], g[:rows, :], g_t, hb[i2][:rows, :], hb_t[i2], D,
                         scr[:rows, :], scr_t, stt[:rows, :], stt_t)
            transposes(c, [hb[i2][:rows, k * 128:(k + 1) * 128] for k in range(8)], hb_t[i2],
                       hTh[:, :, ht * 128:ht * 128 + rows], hTh_t, merge=True)
        for t in range(nt):
            i2 = t % 2
            rmsnorm_tile(c, c.X[:, t, :], c.Xt[t], g[:], g_t, hb[i2][:], hb_t[i2], D, scr, scr_t, stt, stt_t)
            transposes(c, [hb[i2][:, k * 128:(k + 1) * 128] for k in range(8)], hb_t[i2],
                       hT[:, :, t * 128:(t + 1) * 128], hT_t[t])
        wv = c.w_in_o.rearrange("(k p) n -> p k n", p=128)
        wch = [sb(f"m1_w{i}", [128, 8, 4, 128], BF16) for i in range(2)]; wch_t = [Tok(), Tok()]

        def loadc(ci):
            b = ci % 2
            for j in range(4):
                load_w(c, wch[b][:, :, j, :], wv[:, :, j * 512 + ci * 128: j * 512 + (ci + 1) * 128], wch_t[b], 0 + b,
                       merge=(j > 0))

        loadc(0)
        ncks = max(1, ntok // 512)
        cwid = ntok // ncks
        bpc = cwid // 128
        U = [sb("m1_U", [128, nt, 144], F32)] * 2; U_t = [Tok()] * 2
        Hc = sb("m1_Hc", [128, nt, 144], F32); Hc_t = Tok()
        Z, Z_t = U[0], U_t[0]
        S1, S1_t = Hc, Hc_t
        S2 = sb("m1_S2", [128, nt, 144], F32); S2_t = Tok()
        cv = sb("m1_cv", [128, nt, 128], F32); cv_t = Tok()
        pl = sb("m1_pl", [128, nt, 128], BF16); pl_t = Tok()

        def proj(j, b, dst, dst_t, own_only=False, evac="act"):
            for ck in range(ncks):
                M, Mt = next_M(c)
                for k in range(8):
                    kb.op("pe", lambda e, k=k, M=M, ck=ck: e.matmul(M[:, :cwid], lhsT=wch[b][:, k, j, :],
                                                                    rhs=hT[:, k, ck * cwid:(ck + 1) * cwid],
                                                                    start=(k == 0), stop=(k == 7)),
                          reads=[wch_t[b]] + hT_t[ck * bpc:(ck + 1) * bpc], writes=[Mt], merge=(k > 0))
                src = M[:, :cwid].rearrange("p (a b) -> p a b", a=bpc)
                if own_only:
                    d = dst[:, ck * bpc:(ck + 1) * bpc, :]
                else:
                    d = dst[:, ck * bpc:(ck + 1) * bpc, 16:144]
                kb.op("act", lambda e, d=d, src=src: e.copy(out=d, in_=src), reads=[Mt], writes=[dst_t], merge=True)
            if not own_only:
                M, Mt = next_M(c)
                for k in range(8):
                    kb.op("pe", lambda e, k=k, M=M: e.matmul(M[:, :nh], lhsT=wch[b][:, k, j, :], rhs=hTh[:, k, :],
                                                             start=(k == 0), stop=(k == 7)),
                          reads=[wch_t[b], hTh_t], writes=[Mt], merge=(k > 0))
                kb.op("act", lambda e, M=M: e.copy(out=dst[:, :, 0:16], in_=M[:, :nh].rearrange("p (a b) -> p a b", a=nt)),
                      reads=[Mt], writes=[dst_t], merge=True)

        wins = [2, 4, 8, 16]
        for ci in range(4):
            b = ci % 2
            if ci + 1 < 4:
                loadc(ci + 1)
            Ub = U[ci % 2]; Ub_t = U_t[ci % 2]
            proj(1, b, Ub, Ub_t)
            proj(2, b, Hc, Hc_t)
            kb.op("dve", lambda e: e.tensor_tensor(out=Ub[:], in0=Ub[:], in1=Hc[:], op=ALU.mult),
                  reads=[Ub_t, Hc_t], writes=[Ub_t])
            kb.op("dve", lambda e: e.tensor_scalar(out=cv[:], in0=Ub[:, :, 16:144], scalar1=cw_sb[:, ci, 2:3], scalar2=None,
                                                   op0=ALU.mult), reads=[Ub_t, cw_t], writes=[cv_t])
            for kk, sh in ((1, 1), (0, 2)):
                kb.op("dve", lambda e, kk=kk, sh=sh: e.scalar_tensor_tensor(
                    out=cv[:], in0=Ub[:, :, 16 - sh:144 - sh], scalar=cw_sb[:, ci, kk:kk + 1], in1=cv[:],
                    op0=ALU.mult, op1=ALU.add), reads=[Ub_t, cw_t, cv_t], writes=[cv_t])
            for ck in range(ncks):
                M, Mt = next_M(c)
                for k in range(8):
                    kb.op("pe", lambda e, k=k, M=M, ck=ck: e.matmul(M[:, :cwid], lhsT=wch[b][:, k, 0, :],
                                                                    rhs=hT[:, k, ck * cwid:(ck + 1) * cwid],
                                                                    start=(k == 0), stop=(k == 7)),
                          reads=[wch_t[b]] + hT_t[ck * bpc:(ck + 1) * bpc], writes=[Mt], merge=(k > 0))
                kb.op("dve", lambda e, M=M, ck=ck: e.tensor_tensor(
                    out=yT[:, ci, ck * cwid:(ck + 1) * cwid], in0=M[:, :cwid],
                    in1=cv[:, ck * bpc:(ck + 1) * bpc, :].rearrange("p a b -> p (a b)"), op=ALU.mult),
                    reads=[Mt, cv_t], writes=[yT_t[ci]], merge=True)
            proj(3, b, Z, Z_t)
            w = wins[ci]
            cur, cur_t = Z, Z_t
            nxt = [(S1, S1_t), (S2, S2_t)]
            sh = 1
            ni = 0
            lo = 0
            while sh < w:
                dstb, dstb_t = nxt[ni % 2]
                ni += 1
                lo2 = lo + sh
                kb.op("dve", lambda e, cur=cur, dstb=dstb, sh=sh, lo2=lo2: e.tensor_tensor(
                    out=dstb[:, :, lo2:144], in0=cur[:, :, lo2:144], in1=cur[:, :, lo2 - sh:144 - sh], op=ALU.add),
                    reads=[cur_t], writes=[dstb_t])
                cur, cur_t = dstb, dstb_t
                lo = lo2
                sh *= 2
            kb.op("dve", lambda e, cur=cur: e.scalar_tensor_tensor(
                out=pl[:], in0=cur[:, :, 16:144], scalar=1.0 / w, in1=Z[:, :, 16:144], op0=ALU.mult, op1=ALU.subtract),
                reads=[cur_t, Z_t], writes=[pl_t])
            kb.op("dve", lambda e, cur=cur: e.tensor_tensor(out=S1[:, 0, 0:16] if cur is not S1 else S2[:, 0, 0:16],
                                                            in0=cur[:, 0, 16:32], in1=invc[:, ci * 16:(ci + 1) * 16], op=ALU.mult),
                  reads=[cur_t, invc_t], writes=[S1_t if cur is not S1 else S2_t])
            fx = S1 if cur is not S1 else S2
            fx_t = S1_t if cur is not S1 else S2_t
            kb.op("dve", lambda e, fx=fx: e.tensor_tensor(out=pl[:, 0, 0:16], in0=fx[:, 0, 0:16], in1=Z[:, 0, 16:32], op=ALU.subtract),
                  reads=[fx_t, Z_t, pl_t], writes=[pl_t])
            for ck in range(ncks):
                M, Mt = next_M(c)
                kb.op("pe", lambda e, M=M, ck=ck: e.matmul(M[:, :cwid], lhsT=wpool[:, ci, :],
                                                           rhs=pl[:, ck * bpc:(ck + 1) * bpc, :].rearrange("p a b -> p (a b)"),
                                                           start=True, stop=True),
                      reads=[wpool_t, pl_t], writes=[Mt])
                kb.op("act", lambda e, M=M, ck=ck: e.activation(out=yT[:, 4 + ci, ck * cwid:(ck + 1) * cwid], in_=M[:, :cwid],
                                                                func=AF.Identity, scale=ps_sb[:, ci, :]),
                      reads=[Mt, ps_t], writes=[yT_t[4 + ci]], merge=True)
        kb.barrier()
        sin_.close()
        sb = lambda name, shape, dt: c.sb(name, shape, dt, st)
        wo = sb("m1_wo", [128, 8, 1024], BF16); wo_t = Tok()
        load_w(c, wo[:], c.w_out_o.rearrange("(k p) n -> p k n", p=128), wo_t, 5)
        for t in range(nt):
            for ch in range(2):
                M, Mt = next_M(c)
                for k in range(8):
                    kb.op("pe", lambda e, k=k, M=M, t=t, ch=ch: e.matmul(
                        M[:], lhsT=yT[:, k, t * 128:(t + 1) * 128], rhs=wo[:, k, ch * 512:(ch + 1) * 512],
                        start=(k == 0), stop=(k == 7)), reads=[yT_t[k], wo_t], writes=[Mt], merge=(k > 0))
                kb.op("dve", lambda e, M=M, t=t, ch=ch: e.tensor_tensor(out=c.X[:, t, ch * 512:(ch + 1) * 512], in0=M[:],
                                                                      in1=c.X[:, t, ch * 512:(ch + 1) * 512], op=ALU.add),
                      reads=[Mt, c.Xt[t]], writes=[c.Xt[t]])
        kb.barrier()


WINS = (2, 4, 8, 16)


def _common_inputs(inp, layer):
    f = lambda a: np.ascontiguousarray(np.asarray(a, dtype=np.float32))
    m = {"ident": np.eye(128, dtype=np.float32)}
    for nm in ["g_mix", "g_xattn", "g_mem", "g_mlp", "wq_x", "wkv_x", "gq_x", "gk_x", "wo_x", "w_up", "w_down"]:
        m[nm] = f(np.asarray(inp[nm])[layer:layer + 1])
    return m


def _l1_inputs(inp, p):
    f = lambda a: np.ascontiguousarray(np.asarray(a, dtype=np.float32))
    invc = np.zeros((128, 64), np.float32)
    for g, w in enumerate(WINS):
        for j in range(16):
            invc[:, g * 16 + j] = 1.0 / (min(j + 1, w) if p == 0 else w)
    return {
        "w_in_o": f(inp["w_in_o"][0]), "conv_w": f(np.asarray(inp["conv_w"][0]).T.reshape(4, 128, 3).transpose(1, 0, 2).reshape(128, 12)),
        "w_pool": f(inp["w_pool"][0]), "pool_scale": f(np.asarray(inp["pool_scale"][0]).reshape(4, 128).T),
        "w_out_o": f(inp["w_out_o"][0]), "invc": invc,
    }


def _own_blocks(a, p, nt):
    blk = a.reshape((2 * nt, 128) + a.shape[1:])
    return np.ascontiguousarray(blk[p::2].reshape((nt * 128,) + a.shape[1:]))


def prep_l1(x1, inp, nt=NT):
    common = _common_inputs(inp, 1)
    maps = []
    for core in range(8):
        b, p = core // 2, core % 2
        m = dict(common)
        m.update(_l1_inputs(inp, p))
        xs = np.asarray(x1[b], dtype=np.float32)
        m["xo"] = _own_blocks(xs, p, nt)
        xh = np.zeros((nt * 16, D), np.float32)
        for i in range(nt):
            g0 = (2 * i + p) * 128
            if g0 >= 16:
                xh[i * 16:(i + 1) * 16] = xs[g0 - 16:g0]
        if nt * 16 < 256:
            xh = np.concatenate([xh, np.zeros((256 - nt * 16, D), np.float32)], 0)
        m["xh"] = xh
        m["mem"] = np.ascontiguousarray(np.asarray(inp["mem"][b], dtype=np.float32))
        maps.append(m)
    return maps


def gather(res, nt=NT, key="out"):
    out = np.zeros((4, 2 * nt * 128, D), np.float32)
    for core in range(8):
        b, p = core // 2, core % 2
        o = np.asarray(res.results[core][key]).reshape(nt, 128, D)
        out[b].reshape(2 * nt, 128, D)[p::2] = o
    return out


def rope_cast(c, T3, T_tok, H, cs_ap, cs_tok, out3, out_tok, tmp, tmp_tok):
    kb = c.kb
    cosb = cs_ap[:, 0:8].unsqueeze(1).broadcast_to([128, H, 8])
    sinb = cs_ap[:, 8:16].unsqueeze(1).broadcast_to([128, H, 8])
    t3 = tmp[:, :H * 32].rearrange("p (h d) -> p h d", h=H)
    x1 = T3[:, :, 0:8]
    x2 = T3[:, :, 8:16]
    kb.op("dve", lambda e: e.tensor_tensor(out=t3[:, :, 0:8], in0=x1, in1=cosb, op=ALU.mult), reads=[T_tok, cs_tok], writes=[tmp_tok])
    kb.op("dve", lambda e: e.tensor_tensor(out=t3[:, :, 8:16], in0=x2, in1=sinb, op=ALU.mult), reads=[T_tok, cs_tok], writes=[tmp_tok], merge=True)
    kb.op("dve", lambda e: e.tensor_tensor(out=t3[:, :, 16:24], in0=x2, in1=cosb, op=ALU.mult), reads=[T_tok, cs_tok], writes=[tmp_tok], merge=True)
    kb.op("dve", lambda e: e.tensor_tensor(out=t3[:, :, 24:32], in0=x1, in1=sinb, op=ALU.mult), reads=[T_tok, cs_tok], writes=[tmp_tok], merge=True)
    kb.op("act", lambda e: e.copy(out=out3[:, :, 16:64], in_=T3[:, :, 16:64]), reads=[T_tok], writes=[out_tok])
    kb.op("dve", lambda e: e.tensor_tensor(out=out3[:, :, 0:8], in0=t3[:, :, 0:8], in1=t3[:, :, 8:16], op=ALU.subtract),
          reads=[tmp_tok], writes=[out_tok], merge=True)
    kb.op("dve", lambda e: e.tensor_tensor(out=out3[:, :, 8:16], in0=t3[:, :, 16:24], in1=t3[:, :, 24:32], op=ALU.add),
          reads=[tmp_tok], writes=[out_tok], merge=True)


def l0_mixer(c, mode):
    nc, kb, nt = c.nc, c.kb, c.nt
    nb = 2 * nt
    ntok = nt * 128
    xo_v = c.xo.rearrange("(t p) d -> p t d", p=128)
    xt_v = c.xt.rearrange("(t p) d -> p t d", p=128)
    with ExitStack() as so:
        sbo = lambda name, shape, dt: c.sb(name, shape, dt, so)
        g, g_t = load_bc(c, so, "m0_g", c.g_mix[c.li(0)], D)
        gqa, gqa_t = load_bc(c, so, "m0_gqa", c.gq_a[0], 64)
        gka, gka_t = load_bc(c, so, "m0_gka", c.gk_a[0], 64)
        gsub, gsub_t = load_bc(c, so, "m0_gsub", c.g_sub_a[0], 128)
        gkv, gkv_t = load_bc(c, so, "m0_gkv", c.g_kv_b[0], 256)
        gqb, gqb_t = load_bc(c, so, "m0_gqb", c.gq_b[0], 64)
        gkb, gkb_t = load_bc(c, so, "m0_gkb", c.gk_b[0], 64)
        gki, gki_t = load_bc(c, so, "m0_gki", c.g_kidx[0], 64)
        lamv = sbo("m0_lamv", [128, 4, 64], F32); lamv_t = Tok()
        kb.dma("sp", lamv[:].rearrange("p a b -> p (a b)"), c.lamv.rearrange("a b -> (a b)").partition_broadcast(128),
               c.dsem_misc, writes=[lamv_t])
        invf = sbo("m0_invf", [128, 16], F32); invf_t = Tok()
        kb.dma("sp", invf[:], c.invf[:, :], c.dsem_misc, writes=[invf_t])
        posi = sbo("m0_posi", [128, nb], I32); posi_t = Tok()
        kb.dma("sp", posi[:], c.pos[:, :], c.dsem_misc, writes=[posi_t])
        msk = sbo("m0_msk", [128, 2, 128], BF16); msk_t = Tok()
        kb.dma("pool", msk[:, 0, :], c.tri[:, :], c.dsem_misc_sw, writes=[msk_t])
        kb.dma("pool", msk[:, 1, :], c.omask[:, :], c.dsem_misc_sw, writes=[msk_t], merge=True)
        negm = sbo("m0_negm", [128, 2, 128], F32); negm_t = Tok()
        kb.dma("sp", negm[:, 0, :], c.trineg[:, :], c.dsem_misc, writes=[negm_t])
        kb.dma("sp", negm[:, 1, :], c.oneg[:, :], c.dsem_misc, writes=[negm_t], merge=True)
        c.misc += [lamv_t, invf_t, posi_t, msk_t, negm_t]
        kb.flush(c.dsem_misc, c.misc); c.misc = []

        lam_init = 0.8 - 0.6 * float(np.exp(-0.3 * 0))
        lsc = sbo("m0_lsc", [128, 8], F32); lsc_t = Tok()
        lpr = sbo("m0_lpr", [128, 2, 64], F32); lpr_t = Tok()
        kb.op("dve", lambda e: e.tensor_tensor(out=lpr[:, 0, :], in0=lamv[:, 0, :], in1=lamv[:, 1, :], op=ALU.mult), reads=[lamv_t], writes=[lpr_t])
        kb.op("dve", lambda e: e.tensor_tensor(out=lpr[:, 1, :], in0=lamv[:, 2, :], in1=lamv[:, 3, :], op=ALU.mult), reads=[lamv_t], writes=[lpr_t], merge=True)
        kb.op("dve", lambda e: e.reduce_sum(out=lsc[:, 0:2], in_=lpr[:], axis=AX.X), reads=[lpr_t], writes=[lsc_t])
        kb.op("act", lambda e: e.activation(out=lsc[:, 2:4], in_=lsc[:, 0:2], func=AF.Exp), reads=[lsc_t], writes=[lsc_t])
        kb.op("dve", lambda e: e.tensor_tensor(out=lsc[:, 4:5], in0=lsc[:, 3:4], in1=lsc[:, 2:3], op=ALU.subtract), reads=[lsc_t], writes=[lsc_t])
        kb.op("dve", lambda e: e.tensor_scalar(out=lsc[:, 5:6], in0=lsc[:, 4:5], scalar1=-lam_init, scalar2=None, op0=ALU.add),
              reads=[lsc_t], writes=[lsc_t])
        nlam = lsc[:, 5:6]
        kb.op("dve", lambda e: e.tensor_scalar(out=gsub[:], in0=gsub[:], scalar1=1.0 - lam_init, scalar2=None, op0=ALU.mult),
              reads=[gsub_t], writes=[gsub_t])

        cs = sbo("m0_cs", [128, nb, 16], F32); cs_t = Tok()
        OAT = sbo("m0_OAT", [128, 4, ntok], BF16); OAT_t = [Tok() for _ in range(nt)]
        stmp = ExitStack()
        posf = c.sb("m0_posf", [128, nb], F32, stmp); posf_t = Tok()
        ang = c.sb("m0_ang", [128, nb, 16], F32, stmp); ang_t = Tok()
        angi = c.sb("m0_angi", [128, nb, 16], I32, stmp); angi_t = Tok()
        kb.op("dve", lambda e: e.tensor_copy(out=posf[:], in_=posi[:]), reads=[posi_t], writes=[posf_t])
        kb.op("dve", lambda e: e.tensor_tensor(out=ang[:], in0=posf[:].unsqueeze(2).broadcast_to([128, nb, 16]),
                                               in1=invf[:].unsqueeze(1).broadcast_to([128, nb, 16]), op=ALU.mult),
              reads=[posf_t, invf_t], writes=[ang_t])
        kb.op("dve", lambda e: e.tensor_scalar(out=ang[:, :, 0:8], in0=ang[:, :, 0:8], scalar1=float(np.pi / 2), scalar2=None, op0=ALU.add),
              reads=[ang_t], writes=[ang_t])
        kb.op("dve", lambda e: e.tensor_scalar(out=angi[:], in0=ang[:], scalar1=float(1.0 / (2 * np.pi)), scalar2=None, op0=ALU.mult),
              reads=[ang_t], writes=[angi_t])
        kb.op("dve", lambda e: e.tensor_copy(out=cs[:], in_=angi[:]), reads=[angi_t], writes=[cs_t])
        kb.op("dve", lambda e: e.scalar_tensor_tensor(out=ang[:], in0=cs[:], scalar=float(-2 * np.pi), in1=ang[:], op0=ALU.mult, op1=ALU.add),
              reads=[cs_t, ang_t], writes=[ang_t])
        kb.op("dve", lambda e: e.tensor_scalar(out=cs[:], in0=ang[:], scalar1=float(np.pi), scalar2=float(-2 * np.pi), op0=ALU.is_gt, op1=ALU.mult),
              reads=[ang_t], writes=[cs_t])
        kb.op("dve", lambda e: e.tensor_tensor(out=ang[:], in0=ang[:], in1=cs[:], op=ALU.add), reads=[ang_t, cs_t], writes=[ang_t])
        kb.op("dve", lambda e: e.tensor_scalar(out=cs[:], in0=ang[:], scalar1=float(-np.pi), scalar2=float(2 * np.pi), op0=ALU.is_lt, op1=ALU.mult),
              reads=[ang_t], writes=[cs_t])
        kb.op("dve", lambda e: e.tensor_tensor(out=ang[:], in0=ang[:], in1=cs[:], op=ALU.add), reads=[ang_t, cs_t], writes=[ang_t])
        kb.op("act", lambda e: e.activation(out=cs[:], in_=ang[:], func=AF.Sin), reads=[ang_t], writes=[cs_t])
        kb.barrier()
        stmp.close()

        scr = sbo("m0_scr", [128, D], F32); scr_t = Tok()
        stt = sbo("m0_st", [128, 64], F32); stt_t = Tok()
        xin = [sbo(f"m0_xin{i}", [128, D], F32) for i in range(2)]; xin_t = [Tok(), Tok()]
        hb = [sbo("m0_hb", [128, D], BF16)] * 2; hb_t = [Tok()] * 2
        hT = [sbo(f"m0_hT{i}", [128, 8, 128], BF16) for i in range(2)]; hT_t = [Tok(), Tok()]
        pf = sbo("m0_pf", [128, 1280], F32); pf_t = Tok()
        pn = sbo("m0_pn", [128, 12, 64], F32); pn_t = Tok()
        pb = sbo("m0_pb", [128, 12, 64], BF16); pb_t = Tok()
        rtmp = sbo("m0_rtmp", [128, 12 * 32], F32); rtmp_t = Tok()
        state = {"n": 0}

        def load_norm_T(src_v, ti, sc_=None):
            i2 = state["n"] % 2
            state["n"] += 1
            sF, sF_t, stF, stF_t = sc_ if sc_ is not None else (scr, scr_t, stt, stt_t)
            kb.dma("sp", xin[i2][:], src_v[:, ti, :], c.dsem_in[i2], writes=[xin_t[i2]])
            rmsnorm_tile(c, xin[i2][:], xin_t[i2], g[:], g_t, hb[i2][:], hb_t[i2], D, sF, sF_t, stF, stF_t)
            transposes(c, [hb[i2][:, k * 128:(k + 1) * 128] for k in range(8)], hb_t[i2], hT[i2][:], hT_t[i2])
            return i2

        def proj_tok(i2, w, w_t, c0, c1, dst_ap, dst_tok, merge=False):
            M, Mt = next_M(c, 0, 4)
            n = c1 - c0
            for k in range(8):
                kb.op("pe", lambda e, k=k: e.matmul(M[:, :n], lhsT=hT[i2][:, k, :], rhs=w[:, k, c0:c1], start=(k == 0), stop=(k == 7)),
                      reads=[hT_t[i2], w_t], writes=[Mt], merge=(k > 0))
            kb.op("act", lambda e: e.copy(out=dst_ap, in_=M[:, :n]), reads=[Mt], writes=[dst_tok], merge=merge)

        import os
        STOP = os.environ.get("L0_STOP", "")
        if STOP == "setup":
            kb.barrier(); return
        with ExitStack() as sa:
            sba = lambda name, shape, dt: c.sb(name, shape, dt, sa)
            KAT = sba("a_KAT", [128, 4, nb * 128], BF16); KAT_t = [Tok() for _ in range(nb)]
            VA = sba("a_VA", [128, nb, 4, 129], BF16); VA_t = [Tok() for _ in range(nb)]
            wk = sba("a_wk", [128, 8, 1024], BF16); wk_t = Tok()
            wq = sba("a_wq", [128, 8, 512], BF16); wq_t = Tok()
            load_w(c, wk[:], c.wA_k.rearrange("(k p) n -> p k n", p=128), wk_t, 0)
            load_w(c, wq[:], c.wA_q.rearrange("(k p) n -> p k n", p=128), wq_t, 1)
            for s in range(nb):
                kb.op("pool", lambda e, s=s: e.memset(VA[:, s, :, 128:129], 1.0), writes=[VA_t[s]])
            with ExitStack() as sak:
                scrF = c.sb("a_scrF", [128, D], F32, sak); scrF_t = Tok()
                sttF = c.sb("a_sttF", [128, 4], F32, sak); sttF_t = Tok()
                pfh_t = [Tok(), Tok()]

                def akF(s):
                    src_v, ti = (xo_v, s // 2) if s % 2 == 0 else (xt_v, s // 2)
                    i2 = load_norm_T(src_v, ti, (scrF, scrF_t, sttF, sttF_t))
                    h2 = s % 2
                    proj_tok(i2, wk, wk_t, 0, 512, pf[:, h2 * 512:(h2 + 1) * 512], pfh_t[h2])
                    M, Mt = next_M(c, 0, 4)
                    for k in range(8):
                        kb.op("pe", lambda e, k=k, M=M: e.matmul(M[:], lhsT=hT[i2][:, k, :], rhs=wk[:, k, 512:1024], start=(k == 0), stop=(k == 7)),
                              reads=[hT_t[i2], wk_t], writes=[Mt], merge=(k > 0))
                    kb.op("act", lambda e, M=M, s=s: e.copy(out=VA[:, s, :, 0:128], in_=M[:].rearrange("p (h d) -> p h d", h=4)),
                          reads=[Mt], writes=[VA_t[s]], merge=True)

                def akB(s):
                    h2 = s % 2
                    headnorm(c, pf[:, h2 * 512:(h2 + 1) * 512].rearrange("p (h d) -> p h d", h=8), pfh_t[h2], 8, 64, gka[:], gka_t,
                             pn[:, 0:8, :], pn_t, scr, scr_t, stt, stt_t)
                    rope_cast(c, pn[:, 0:8, :], pn_t, 8, cs[:, s, :], cs_t, pb[:, 0:8, :], pb_t, rtmp, rtmp_t)
                    pb2 = pb[:, 0:8, :].rearrange("p h d -> p (h d)")
                    transposes(c, [pb2[:, h * 128:(h + 1) * 128] for h in range(4)], pb_t, KAT[:, :, s * 128:(s + 1) * 128], KAT_t[s])

                pipeline(nb, [akF, akB])
                kb.barrier()
            if STOP == "AK":
                kb.barrier(); return
            QAT = [sba(f"a_QAT{i}", [128, 4, 2, 128], BF16) for i in range(2)]; QAT_t = [Tok(), Tok()]
            for i_ in range(2):
                kb.op("pool", lambda e, i_=i_: e.memset(QAT[i_][:], 0.0), writes=[QAT_t[i_]])
            PT = [sba(f"a_PT{i}", [128, 4, 128], BF16) for i in range(3)]; PT_t = [Tok() for _ in range(3)]
            oa2 = [sba(f"a_oa{i}", [128, 4, 128], F32) for i in range(2)]; oa2_t = [Tok(), Tok()]
            oab = sba("a_oab", [128, 4, 128], BF16); oab_t = Tok()
            tq = sba("a_tq", [128, 128], F32); tq_t = Tok()
            rec = sba("a_rec", [128, 8], F32); rec_t = Tok()
            sc = 64.0 ** -0.5
            astate = {"pti": 0}

            def aqF(i):
                i2 = load_norm_T(xo_v, i)
                proj_tok(i2, wq, wq_t, 0, 512, pf[:, 0:512], pf_t)
                headnorm(c, pf[:, 0:512].rearrange("p (h d) -> p h d", h=8), pf_t, 8, 64, gqa[:], gqa_t,
                         pn[:, 0:8, :], pn_t, scr, scr_t, stt, stt_t)
                rope_cast(c, pn[:, 0:8, :], pn_t, 8, cs[:, 2 * i, :], cs_t, pb[:, 0:8, :], pb_t, rtmp, rtmp_t)
                pb2 = pb[:, 0:8, :].rearrange("p h d -> p (h d)")
                qi2 = i % 2
                transposes(c, [pb2[:, h * 128:(h + 1) * 128] for h in range(4)], pb_t, QAT[qi2], QAT_t[qi2], halves=True)

            def aqG(i):
                qi2 = i % 2
                oa, oa_t = oa2[i % 2], oa2_t[i % 2]
                nslots = 2 * i + 2
                for hp in range(2):
                    O1, O1t = c.M[4], c.Mt[4]
                    O2, O2t = c.M[5], c.Mt[5]
                    def emit_SA(s):
                        M, Mt = next_M(c, 0, 4)
                        for u in range(4):
                            h = 2 * hp + u // 2
                            cc = u % 2
                            kb.op("pe", lambda e, M=M, u=u, h=h, cc=cc, s=s: e.matmul(
                                M[:, u * 128:(u + 1) * 128], lhsT=KAT[:, h, s * 128:(s + 1) * 128],
                                rhs=QAT[qi2][:, h, cc, :], start=True, stop=True),
                                reads=[KAT_t[s], QAT_t[qi2]], writes=[Mt], merge=(u > 0))
                        return M, Mt

                    curS = emit_SA(0)
                    for s in range(nslots):
                        M, Mt = curS
                        if s + 1 < nslots:
                            curS = emit_SA(s + 1)
                        p3 = astate["pti"] % 3
                        astate["pti"] += 1
                        kb.op("act", lambda e, M=M, p3=p3: e.activation(out=PT[p3][:], in_=M[:].rearrange("p (a b) -> p a b", a=4),
                                                                     func=AF.Exp, scale=sc), reads=[Mt], writes=[PT_t[p3]])
                        if s >= nslots - 2:
                            mi_ = 0 if s == nslots - 2 else 1
                            kb.op("dve", lambda e, p3=p3, mi_=mi_: e.tensor_tensor(
                                out=PT[p3][:], in0=PT[p3][:], in1=msk[:, mi_, :].unsqueeze(1).broadcast_to([128, 4, 128]), op=ALU.mult),
                                reads=[PT_t[p3], msk_t], writes=[PT_t[p3]])
                        for u in range(4):
                            h = 2 * hp + u // 2
                            O, Ot = (O1, O1t) if u < 3 else (O2, O2t)
                            off = (u % 3) * 129
                            kb.op("pe", lambda e, O=O, off=off, u=u, h=h, s=s, p3=p3: e.matmul(
                                O[:, off:off + 129], lhsT=PT[p3][:, u, :], rhs=VA[:, s, h, :], start=(s == 0 and u in (0, 3)),
                                stop=(s == nslots - 1), skip_group_check=True),
                                reads=[PT_t[p3], VA_t[s]], writes=[Ot], merge=True)
                    for u in range(4):
                        O, Ot = (O1, O1t) if u < 3 else (O2, O2t)
                        off = (u % 3) * 129
                        kb.op("dve", lambda e, O=O, off=off, u=u: e.reciprocal(out=rec[:, u:u + 1], in_=O[:, off + 128:off + 129]),
                              reads=[Ot], writes=[rec_t], merge=(u > 0))
                    kb.op("dve", lambda e: e.tensor_scalar(out=rec[:, 4:8], in0=rec[:, 0:4], scalar1=nlam, scalar2=None, op0=ALU.mult),
                          reads=[rec_t, lsc_t], writes=[rec_t])
                    for hh in range(2):
                        h = 2 * hp + hh
                        u0, u1 = 2 * hh, 2 * hh + 1
                        Oa, Oat = (O1, O1t) if u0 < 3 else (O2, O2t)
                        Ob, Obt = (O1, O1t) if u1 < 3 else (O2, O2t)
                        offa, offb = (u0 % 3) * 129, (u1 % 3) * 129
                        kb.op("dve", lambda e, Ob=Ob, offb=offb, u1=u1: e.tensor_scalar(out=tq[:], in0=Ob[:, offb:offb + 128],
                                                                                      scalar1=rec[:, 4 + u1:5 + u1], scalar2=None, op0=ALU.mult),
                              reads=[Obt, rec_t], writes=[tq_t])
                        kb.op("dve", lambda e, Oa=Oa, offa=offa, u0=u0, h=h: e.scalar_tensor_tensor(
                            out=oa[:, h, :], in0=Oa[:, offa:offa + 128], scalar=rec[:, u0:u0 + 1], in1=tq[:], op0=ALU.mult, op1=ALU.add),
                            reads=[Oat, rec_t, tq_t], writes=[oa_t], merge=True)

            def aqH(i):
                oa, oa_t = oa2[i % 2], oa2_t[i % 2]
                headnorm(c, oa[:], oa_t, 4, 128, gsub[:], gsub_t, oab[:], oab_t, scr, scr_t, stt, stt_t)
                transposes(c, [oab[:, h, :] for h in range(4)], oab_t, OAT[:, :, i * 128:(i + 1) * 128], OAT_t[i])

            pipeline(nt, [aqF, aqG, aqH])
            kb.barrier()

        if STOP == "A":
            kb.barrier(); return
        with ExitStack() as sB:
            sbb = lambda name, shape, dt: c.sb(name, shape, dt, sB)
            KBT = sbb("b_KBT", [128, 4, nb * 128], BF16); KBT_t = [Tok() for _ in range(nb)]
            VB = sbb("b_VB", [128, nb, 8, 65], BF16); VB_t = [Tok() for _ in range(nb)]
            KIT = sbb("b_KIT", [128, nb * 128], BF16); KIT_t = [Tok() for _ in range(nb)]
            for s in range(nb):
                kb.op("pool", lambda e, s=s: e.memset(VB[:, s, :, 64:65], 1.0), writes=[VB_t[s]])
            with ExitStack() as sk:
                sbk = lambda name, shape, dt: c.sb(name, shape, dt, sk)
                wk = sbk("b_wk", [128, 8, 320], BF16); wk_t = Tok()
                wup = sbk("b_wup", [128, 2, 1024], BF16); wup_t = Tok()
                load_w(c, wk[:], c.wB_k.rearrange("(k p) n -> p k n", p=128), wk_t, 0)
                load_w(c, wup[:], c.w_kv_up.rearrange("(k p) n -> p k n", p=128), wup_t, 1)
                cb = sbk("b_cb", [128, 256], BF16); cb_t = Tok()
                cT = sbk("b_cT", [128, 2, 128], BF16); cT_t = Tok()
                scrF = sbk("b_scrF", [128, D], F32); scrF_t = Tok()
                sttF = sbk("b_sttF", [128, 4], F32); sttF_t = Tok()
                pfa_t = [Tok(), Tok()]
                pfk_t = Tok()

                def bkF(s):
                    src_v, ti = (xo_v, s // 2) if s % 2 == 0 else (xt_v, s // 2)
                    i2 = load_norm_T(src_v, ti, (scrF, scrF_t, sttF, sttF_t))
                    h2 = s % 2
                    proj_tok(i2, wk, wk_t, 0, 320, pf[:, h2 * 320:(h2 + 1) * 320], pfa_t[h2])

                def bkB(s):
                    h2 = s % 2
                    o = h2 * 320
                    rmsnorm_tile(c, pf[:, o:o + 256], pfa_t[h2], gkv[:], gkv_t, cb[:], cb_t, 256, scr, scr_t, stt, stt_t)
                    transposes(c, [cb[:, k * 128:(k + 1) * 128] for k in range(2)], cb_t, cT[:], cT_t)
                    headnorm(c, pf[:, o + 256:o + 320].rearrange("p (h d) -> p h d", h=1), pfa_t[h2], 1, 64, gki[:], gki_t,
                             pn[:, 8:9, :], pn_t, scr, scr_t, stt, stt_t)
                    M, Mt = next_M(c, 0, 4)
                    for k in range(2):
                        kb.op("pe", lambda e, k=k, M=M: e.matmul(M[:], lhsT=cT[:, k, :], rhs=wup[:, k, 0:512], start=(k == 0), stop=(k == 1)),
                              reads=[cT_t, wup_t], writes=[Mt], merge=(k > 0))
                    kb.op("act", lambda e, M=M: e.copy(out=pf[:, 768:1280], in_=M[:]), reads=[Mt], writes=[pfk_t])
                    M2, M2t = next_M(c, 0, 4)
                    for k in range(2):
                        kb.op("pe", lambda e, k=k, M2=M2: e.matmul(M2[:], lhsT=cT[:, k, :], rhs=wup[:, k, 512:1024], start=(k == 0), stop=(k == 1)),
                              reads=[cT_t, wup_t], writes=[M2t], merge=(k > 0))
                    kb.op("act", lambda e, M2=M2, s=s: e.copy(out=VB[:, s, :, 0:64], in_=M2[:].rearrange("p (h d) -> p h d", h=8)),
                          reads=[M2t], writes=[VB_t[s]], merge=True)
                    headnorm(c, pf[:, 768:1280].rearrange("p (h d) -> p h d", h=8), pfk_t, 8, 64, gkb[:], gkb_t,
                             pn[:, 0:8, :], pn_t, scr, scr_t, stt, stt_t)
                    rope_cast(c, pn[:, 0:9, :], pn_t, 9, cs[:, s, :], cs_t, pb[:, 0:9, :], pb_t, rtmp, rtmp_t)
                    kb.op("act", lambda e: e.copy(out=pb[:, 9, :], in_=pb[:, 8, :]), reads=[pb_t], writes=[pb_t])
                    pb2 = pb[:, 0:10, :].rearrange("p h d -> p (h d)")
                    transposes(c, [pb2[:, h * 128:(h + 1) * 128] for h in range(4)], pb_t, KBT[:, :, s * 128:(s + 1) * 128], KBT_t[s])
                    transposes(c, [pb2[:, 512:640]], pb_t, KIT[:, s * 128:(s + 1) * 128], KIT_t[s])

                pipeline(nb, [bkF, bkB])
                kb.barrier()
            if STOP == "BK":
                kb.barrier(); return
            with ExitStack() as sq:
                sbq = lambda name, shape, dt: c.sb(name, shape, dt, sq)
                wq = sbq("b_wq", [128, 8, 772], BF16); wq_t = Tok()
                wo = sbq("b_wo", [128, 8, 1024], BF16); wo_t = Tok()
                load_w(c, wq[:], c.wB_q.rearrange("(k p) n -> p k n", p=128), wq_t, 0)
                load_w(c, wo[:], c.w_out_e.rearrange("(k p) n -> p k n", p=128), wo_t, 1)
                QBT2 = [sbq(f"b_QBT{i}", [128, 4, 2, 128], BF16) for i in range(2)]; QBT2_t = [Tok(), Tok()]
                QIT = sbq("b_QIT", [128, 2, 2, 128], BF16); QIT_t = Tok()
                for i_ in range(2):
                    kb.op("pool", lambda e, i_=i_: e.memset(QBT2[i_][:], 0.0), writes=[QBT2_t[i_]])
                kb.op("pool", lambda e: e.memset(QIT[:], 0.0), writes=[QIT_t])
                wsc = sbq("b_wsc", [128, 4], F32); wsc_t = Tok()
                isc = sbq("b_isc", [128, nb * 128], F32); isc_t = Tok()
                mkb = sbq("b_mkb", [128, nb * 128], BF16); mkb_t = Tok()
                mkT2 = [sbq("b_mkT", [128, nb * 128], BF16)] * 2; mkT2_t = [Tok()] * 2
                Rr = [sbq(f"b_R{i}", [128, 512], F32) for i in range(2)]; Rr_t = [Tok(), Tok()]
                PT8 = [sbq(f"b_PT{i}", [128, 8, 128], BF16) for i in range(2)]; PT8_t = [Tok(), Tok()]
                bs = sbq("b_bs", [128, 16], F32); bs_t = Tok()
                negb = sbq("b_negb", [128, 1], F32); negb_t = Tok()
                kb.op("pool", lambda e: e.memset(negb[:], -30000.0), writes=[negb_t])
                Rk = sbq("b_Rk", [128, BIS_ITERS + 1], F32); Rk_t = Tok()
                pw2 = sbq("b_pw2", [128, BIS_ITERS + 1], F32); pw2_t = Tok()
                for k_ in range(BIS_ITERS + 1):
                    kb.op("pool", lambda e, k_=k_: e.memset(pw2[:, k_:k_ + 1], float(2.0 ** -k_)), writes=[pw2_t], merge=True)
                recb = sbq("b_recb", [128, 8, 1], F32); recb_t = Tok()
                ob = sbq("b_ob", [128, 8, 64], BF16); ob_t = Tok()
                OBT = sbq("b_OBT", [128, 4, 128], BF16); OBT_t = Tok()
                sc = 64.0 ** -0.5
                bst = {"ri": 0, "pi8": 0, "i2": {}}

                def bqF(i):
                    i2 = load_norm_T(xo_v, i)
                    bst["i2"][i] = i2
                    QBT, QBT_t = QBT2[i % 2], QBT2_t[i % 2]
                    mkT, mkT_t = mkT2[i % 2], mkT2_t[i % 2]
                    proj_tok(i2, wq, wq_t, 0, 512, pf[:, 0:512], pf_t)
                    proj_tok(i2, wq, wq_t, 512, 772, pf[:, 512:772], pf_t, merge=True)
                    headnorm(c, pf[:, 0:512].rearrange("p (h d) -> p h d", h=8), pf_t, 8, 64, gqb[:], gqb_t,
                             pn[:, 0:8, :], pn_t, scr, scr_t, stt, stt_t)
                    kb.op("act", lambda e: e.copy(out=pn[:, 8:12, :], in_=pf[:, 512:768].rearrange("p (h d) -> p h d", h=4)),
                          reads=[pf_t], writes=[pn_t], merge=True)
                    kb.op("dve", lambda e: e.tensor_scalar(out=wsc[:], in0=pf[:, 768:772], scalar1=1.0 / 16.0, scalar2=None, op0=ALU.mult),
                          reads=[pf_t], writes=[wsc_t])
                    rope_cast(c, pn[:, 0:12, :], pn_t, 12, cs[:, 2 * i, :], cs_t, pb[:, 0:12, :], pb_t, rtmp, rtmp_t)
                    pb2 = pb[:, 0:12, :].rearrange("p h d -> p (h d)")
                    transposes(c, [pb2[:, h * 128:(h + 1) * 128] for h in range(4)], pb_t, QBT, QBT_t, halves=True)
                    transposes(c, [pb2[:, 512 + h * 128:512 + (h + 1) * 128] for h in range(2)], pb_t, QIT, QIT_t, halves=True)
                    nslots = 2 * i + 2
                    nk = nslots * 128
                    for c0 in range(0, nk, 512):
                        wd = min(512, nk - c0)
                        sl = list(range(c0 // 128, (c0 + wd) // 128))
                        for h in range(4):
                            M, Mt = next_M(c, 0, 4)
                            pr = (h % 2) * 64
                            kb.op("pe", lambda e, M=M, h=h, pr=pr, c0=c0, wd=wd: e.matmul(
                                M[:, :wd], lhsT=QIT[:, h // 2, h % 2, :], rhs=KIT[:, c0:c0 + wd], start=True, stop=True),
                                reads=[QIT_t] + [KIT_t[s] for s in sl], writes=[Mt])
                            r = bst["ri"] % 2
                            bst["ri"] += 1
                            kb.op("act", lambda e, M=M, r=r, wd=wd: e.activation(out=Rr[r][:, :wd], in_=M[:, :wd], func=AF.Relu),
                                  reads=[Mt], writes=[Rr_t[r]])
                            if h == 0:
                                kb.op("dve", lambda e, r=r, c0=c0, wd=wd: e.tensor_scalar(out=isc[:, c0:c0 + wd], in0=Rr[r][:, :wd],
                                                                                        scalar1=wsc[:, 0:1], scalar2=None, op0=ALU.mult),
                                      reads=[Rr_t[r], wsc_t], writes=[isc_t], merge=True)
                            else:
                                kb.op("dve", lambda e, r=r, c0=c0, wd=wd, h=h: e.scalar_tensor_tensor(
                                    out=isc[:, c0:c0 + wd], in0=Rr[r][:, :wd], scalar=wsc[:, h:h + 1], in1=isc[:, c0:c0 + wd],
                                    op0=ALU.mult, op1=ALU.add), reads=[Rr_t[r], wsc_t, isc_t], writes=[isc_t], merge=True)
                    kb.op("dve", lambda e: e.reduce_max(out=bs[:, 0:1], in_=isc[:, :nk], axis=AX.X, apply_absolute_value=True),
                          reads=[isc_t], writes=[bs_t])
                    kb.op("dve", lambda e: e.tensor_scalar(out=bs[:, 1:2], in0=bs[:, 0:1], scalar1=1.001, scalar2=1e-3, op0=ALU.mult, op1=ALU.add),
                          reads=[bs_t], writes=[bs_t])
                    kb.op("dve", lambda e: e.tensor_scalar(out=Rk[:], in0=pw2[:], scalar1=bs[:, 1:2], scalar2=None, op0=ALU.mult),
                          reads=[bs_t, pw2_t], writes=[Rk_t])
                    kb.op("dve", lambda e: e.memset(bs[:, 2:3], 0.0), writes=[bs_t], merge=True)
                    for mi_, s in ((0, nslots - 2), (1, nslots - 1)):
                        kb.op("dve", lambda e, mi_=mi_, s=s: e.tensor_tensor(out=isc[:, s * 128:(s + 1) * 128], in0=isc[:, s * 128:(s + 1) * 128],
                                                                         in1=negm[:, mi_, :], op=ALU.add),
                              reads=[isc_t, negm_t], writes=[isc_t])
                    mid, cnt, tp_, thr = bs[:, 2:3], bs[:, 3:4], bs[:, 4:5], bs[:, 5:6]
                    for it in range(BIS_ITERS):
                        kb.op("dve", lambda e: e.tensor_scalar(out=mkb[:, :nk], in0=isc[:, :nk], scalar1=mid, scalar2=None,
                                                               op0=ALU.is_gt, op1=ALU.add, accum_out=cnt),
                              reads=[isc_t, bs_t], writes=[mkb_t, bs_t])
                        kb.op("dve", lambda e: e.tensor_scalar(out=tp_, in0=cnt, scalar1=float(c.topk) - 0.5, scalar2=0.5,
                                                               op0=ALU.is_ge, op1=ALU.subtract), reads=[bs_t], writes=[bs_t])
                        kb.op("dve", lambda e, it=it: e.scalar_tensor_tensor(out=mid, in0=tp_, scalar=Rk[:, it:it + 1], in1=mid,
                                                                             op0=ALU.mult, op1=ALU.add), reads=[bs_t, Rk_t], writes=[bs_t])
                    kb.op("dve", lambda e: e.scalar_tensor_tensor(out=thr, in0=Rk[:, BIS_ITERS:BIS_ITERS + 1], scalar=-1.0, in1=mid,
                                                                  op0=ALU.mult, op1=ALU.add), reads=[bs_t, Rk_t], writes=[bs_t])
                    kb.op("dve", lambda e: e.tensor_scalar(out=mkb[:, :nk], in0=isc[:, :nk], scalar1=thr, scalar2=None, op0=ALU.is_gt),
                          reads=[isc_t, bs_t], writes=[mkb_t])

                def bqH(i):
                    mkT, mkT_t = mkT2[i % 2], mkT2_t[i % 2]
                    nslots = 2 * i + 2
                    for s0 in range(0, nslots, 8):
                        n8 = min(8, nslots - s0)
                        T, Tt = next_T(c)
                        for j in range(n8):
                            kb.op("pe", lambda e, j=j, T=T: e.transpose(out=T[:, j * 128:(j + 1) * 128], in_=mkb[:, (s0 + j) * 128:(s0 + j + 1) * 128],
                                                                      identity=c.identb[:]), reads=[mkb_t, c.ident_t], writes=[Tt], merge=(j > 0))
                        kb.op("act", lambda e, T=T, n8=n8, s0=s0: e.activation(out=mkT[:, s0 * 128:(s0 + n8) * 128], in_=T[:, :n8 * 128],
                                                                             func=AF.Identity, scale=30000.0, bias=negb[:, 0:1]),
                              reads=[Tt, negb_t], writes=[mkT_t], merge=True)

                def bqG(i):
                    i2 = bst["i2"][i]
                    QBT, QBT_t = QBT2[i % 2], QBT2_t[i % 2]
                    mkT, mkT_t = mkT2[i % 2], mkT2_t[i % 2]
                    nslots = 2 * i + 2
                    O1, O1t = c.M[4], c.Mt[4]
                    O2, O2t = c.M[5], c.Mt[5]
                    def emit_SB(s):
                        Ms = [next_M(c, 0, 4), next_M(c, 0, 4)]
                        for u in range(8):
                            M, Mt = Ms[u // 4]
                            kb.op("pe", lambda e, M=M, u=u, s=s: e.matmul(
                                M[:, (u % 4) * 128:(u % 4 + 1) * 128], lhsT=KBT[:, u // 2, s * 128:(s + 1) * 128],
                                rhs=QBT[:, u // 2, u % 2, :], start=(u % 4 == 0), stop=False, skip_group_check=True),
                                reads=[KBT_t[s], QBT_t], writes=[Mt], merge=(u % 4 > 0))
                        for j in range(2):
                            M, Mt = Ms[j]
                            kb.op("pe", lambda e, M=M, s=s: e.matmul(
                                M[:].rearrange("p (a b) -> p a b", a=4), lhsT=c.identb[:],
                                rhs=mkT[:, s * 128:(s + 1) * 128].unsqueeze(1).broadcast_to([128, 4, 128]),
                                start=False, stop=True, skip_group_check=True),
                                reads=[mkT_t, c.ident_t], writes=[Mt], merge=True)
                        return Ms

                    curS = emit_SB(0)
                    for s in range(nslots):
                        Ms = curS
                        if s + 1 < nslots:
                            curS = emit_SB(s + 1)
                        p8 = bst["pi8"] % 2
                        bst["pi8"] += 1
                        for j in range(2):
                            M, Mt = Ms[j]
                            kb.op("act", lambda e, M=M, j=j, p8=p8: e.activation(out=PT8[p8][:, j * 4:(j + 1) * 4, :],
                                                                             in_=M[:].rearrange("p (a b) -> p a b", a=4), func=AF.Exp, scale=sc),
                                  reads=[Mt], writes=[PT8_t[p8]], merge=(j > 0))
                        for u in range(8):
                            O, Ot = (O1, O1t) if u < 4 else (O2, O2t)
                            off = (u % 4) * 65
                            kb.op("pe", lambda e, O=O, off=off, u=u, s=s, p8=p8: e.matmul(
                                O[:, off:off + 65], lhsT=PT8[p8][:, u, :], rhs=VB[:, s, u, :], start=(s == 0 and u in (0, 4)),
                                stop=(s == nslots - 1), skip_group_check=True),
                                reads=[PT8_t[p8], VB_t[s]], writes=[Ot], merge=True)
                    for j, (O, Ot) in enumerate(((O1, O1t), (O2, O2t))):
                        O3 = O[:, 0:260].rearrange("p (u d) -> p u d", u=4)
                        kb.op("dve", lambda e, O3=O3, j=j: e.reciprocal(out=recb[:, j * 4:(j + 1) * 4, :], in_=O3[:, :, 64:65]),
                              reads=[Ot], writes=[recb_t], merge=(j > 0))
                    for j, (O, Ot) in enumerate(((O1, O1t), (O2, O2t))):
                        O3 = O[:, 0:260].rearrange("p (u d) -> p u d", u=4)
                        kb.op("dve", lambda e, O3=O3, j=j: e.tensor_tensor(out=ob[:, j * 4:(j + 1) * 4, :], in0=O3[:, :, 0:64],
                                                                        in1=recb[:, j * 4:(j + 1) * 4, :].broadcast_to([128, 4, 64]), op=ALU.mult),
                              reads=[Ot, recb_t], writes=[ob_t], merge=(j > 0))
                    ob2 = ob[:].rearrange("p h d -> p (h d)")
                    transposes(c, [ob2[:, h * 128:(h + 1) * 128] for h in range(4)], ob_t, OBT[:], OBT_t)
                    xi = i % 2
                    for ch in range(2):
                        M, Mt = next_M(c, 0, 4)
                        for k in range(8):
                            if k < 4:
                                kb.op("pe", lambda e, M=M, k=k, ch=ch: e.matmul(M[:], lhsT=OAT[:, k, i * 128:(i + 1) * 128],
                                                                                rhs=wo[:, k, ch * 512:(ch + 1) * 512], start=(k == 0), stop=False),
                                      reads=[OAT_t[i], wo_t], writes=[Mt], merge=(k > 0))
                            else:
                                kb.op("pe", lambda e, M=M, k=k, ch=ch: e.matmul(M[:], lhsT=OBT[:, k - 4, :],
                                                                                rhs=wo[:, k, ch * 512:(ch + 1) * 512], start=False, stop=(k == 7)),
                                      reads=[OBT_t, wo_t], writes=[Mt], merge=True)
                        kb.op("dve", lambda e, M=M, ch=ch: e.tensor_tensor(out=xin[i2][:, ch * 512:(ch + 1) * 512], in0=M[:],
                                                                         in1=xin[i2][:, ch * 512:(ch + 1) * 512], op=ALU.add),
                              reads=[Mt, xin_t[i2]], writes=[xin_t[i2]])
                    kb.dma("sp", c.x1s_v[:, i, :], xin[i2][:], c.dsem_x1[xi], reads=[xin_t[i2]], writes=[c.x1s_t[i]])

                bqF(0)
                for i in range(nt):
                    bqH(i)
                    if i + 1 < nt:
                        bqF(i + 1)
                    bqG(i)
            kb.barrier()


def _l0_inputs(inp, p):
    f = lambda a: np.ascontiguousarray(np.asarray(a, dtype=np.float32))
    w = np.asarray(inp["w_in_e"][0], dtype=np.float32)
    qa, ka, va, qb, ckv, qi, ki, wi = np.split(w, np.cumsum([512, 512, 512, 512, 256, 256, 64, 4])[:-1], axis=1)
    k = np.arange(128)
    tri = (k[:, None] <= k[None, :]).astype(np.float32)
    inv = (500000.0 ** (-np.arange(0, 16, 2, dtype=np.float32) / 16)).astype(np.float32)
    return {
        "invf": np.tile(np.concatenate([inv, inv])[None, :], (128, 1)).astype(np.float32),
        "tri": tri, "omask": np.full((128, 128), float(p), np.float32),
        "trineg": np.where(tri.T > 0, 0.0, NEG).astype(np.float32),
        "oneg": np.full((128, 128), 0.0 if p == 1 else NEG, np.float32),
        "lamv": f(np.stack([inp["lam_q1"][0], inp["lam_k1"][0], inp["lam_q2"][0], inp["lam_k2"][0]])),
        "gq_a": f(inp["gq_a"]), "gk_a": f(inp["gk_a"]), "g_sub_a": f(inp["g_sub_a"]), "g_kv_b": f(inp["g_kv_b"]),
        "gq_b": f(inp["gq_b"]), "gk_b": f(inp["gk_b"]), "g_kidx": f(inp["g_kidx"]),
        "wA_k": f(np.concatenate([ka, va], 1)), "wA_q": f(qa),
        "wB_k": f(np.concatenate([ckv, ki], 1)), "wB_q": f(np.concatenate([qb, qi, wi], 1)),
        "w_kv_up": f(inp["w_kv_up_b"][0]), "w_out_e": f(inp["w_out_e"][0]),
    }


def prep_l0(inp, nt=NT, mixonly=False, layers=(0,)):
    f = lambda a: np.ascontiguousarray(np.asarray(a, dtype=np.float32))
    maps = []
    x = np.asarray(inp["x"], dtype=np.float32)
    pos = np.asarray(inp["positions"]).astype(np.int32)
    ls = list(layers)
    for core in range(8):
        b, p = core // 2, core % 2
        m = {"ident": np.eye(128, dtype=np.float32), "g_mix": f(np.asarray(inp["g_mix"])[ls])}
        if not mixonly:
            for nm in ["g_xattn", "g_mem", "g_mlp", "wq_x", "wkv_x", "gq_x", "gk_x", "wo_x", "w_up", "w_down"]:
                m[nm] = f(np.asarray(inp[nm])[ls])
            m["mem"] = f(inp["mem"][b])
        m.update(_l0_inputs(inp, p))
        m["xo"] = _own_blocks(x[b], p, nt)
        m["xt"] = _own_blocks(x[b], 1 - p, nt)
        pb = pos[b].reshape(2 * nt, 128)
        slots = np.empty((2 * nt, 128), np.int32)
        slots[0::2] = pb[p::2]
        slots[1::2] = pb[(1 - p)::2]
        m["pos"] = np.ascontiguousarray(slots.T)
        maps.append(m)
    return maps


def prep_all(inp, nt=NT):
    maps = prep_l0(inp, nt, mixonly=False, layers=(0, 1))
    for core in range(8):
        p = core % 2
        maps[core].update(_l1_inputs(inp, p))
        hs = np.zeros((128, 2), np.float32)
        hs[:, 0] = 1.0 if p == 1 else 0.0
        hs[:, 1] = 1.0 if p == 0 else 0.0
        maps[core]["hsel"] = hs
    return maps


def kernel(**inputs):
    inp = {k: np.asarray(v) for k, v in inputs.items()}
    nc, _ = build("ALL", NT)
    res = run_bass_kernel_spmd(nc, prep_all(inp, NT), core_ids=list(range(8)))
    return gather(res, NT)
```
